# Optimizing a Trainium2 kernel written in Bass

```python
import math
import jax, jax.numpy as jnp
from jax import lax
import numpy as np

D_MODEL = 1024
BATCH = 16
SEQ = 2048
DEPTH = 4

A_HEADS = 8
A_HEAD_DIM = 64
A_WIDTH = A_HEADS * A_HEAD_DIM
IDX_HEADS = 4
IDX_DIM = 64
TOPK_MAX = 256
B_HEADS = 8
B_NOPE_DIM = 64
B_ROPE_DIM = 32
B_QK_DIM = B_NOPE_DIM + B_ROPE_DIM
B_V_DIM = 64
B_WIDTH = B_HEADS * B_V_DIM
Q_LORA = 256
KV_LORA = 128
ROPE_THETA = 10000.0
REL_BUCKETS = 32
REL_MAX_DIST = 128
N_BRANCHES = 2
D_FF = 4 * D_MODEL
Q_BLOCK = 128
EPS = 1e-6
NEG_INF = -1e30
IN_SIZES = (A_WIDTH, A_HEAD_DIM, A_HEAD_DIM, IDX_HEADS * IDX_DIM, IDX_DIM, IDX_HEADS,
            Q_LORA, KV_LORA, B_ROPE_DIM, N_BRANCHES * D_MODEL)
D_IN = sum(IN_SIZES)

kernel_name = "hybrid_dsa_mla_gated_trunk"


def rmsnorm(x, g):
    xf = x.astype(jnp.float32)
    y = xf * lax.rsqrt(jnp.mean(xf * xf, axis=-1, keepdims=True) + EPS)
    return (y * g.astype(jnp.float32)).astype(x.dtype)


def split_cols(z, sizes):
    cuts = [int(c) for c in np.cumsum(sizes)[:-1]]
    return jnp.split(z, cuts, axis=-1)


def rope_tables(positions):
    inv_freq = ROPE_THETA ** (-jnp.arange(0, B_ROPE_DIM, 2, dtype=jnp.float32) / B_ROPE_DIM)
    ang = positions.astype(jnp.float32)[..., None] * inv_freq
    return jnp.cos(ang), jnp.sin(ang)


def apply_rope(x, cos, sin):
    x1, x2 = jnp.split(x, 2, axis=-1)
    cos = cos.astype(x.dtype)
    sin = sin.astype(x.dtype)
    return jnp.concatenate([x1 * cos - x2 * sin, x1 * sin + x2 * cos], axis=-1)


def rel_bucket(dist):
    max_exact = REL_BUCKETS // 2
    n = jnp.maximum(dist, 0)
    nf = jnp.maximum(n.astype(jnp.float32), 1.0)
    log_b = max_exact + (jnp.log(nf / max_exact) / math.log(REL_MAX_DIST / max_exact)
                         * (REL_BUCKETS - max_exact)).astype(jnp.int32)
    return jnp.where(n < max_exact, n, jnp.minimum(log_b, REL_BUCKETS - 1))


def take_rows(arr, idx):
    return jax.vmap(lambda a, i: a[i])(arr, idx)


def dsa_sparse_attention(q, k, v, iq, ik, iw, positions, rel_table):
    bsz, L = q.shape[0], q.shape[1]
    k_top = min(TOPK_MAX, L // 4)
    key_idx = jnp.arange(L)
    ikf = ik.astype(jnp.float32)

    def one_block(bi):
        start = bi * Q_BLOCK
        qb = lax.dynamic_slice_in_dim(q, start, Q_BLOCK, axis=1)
        iqb = lax.dynamic_slice_in_dim(iq, start, Q_BLOCK, axis=1).astype(jnp.float32)
        iwb = lax.dynamic_slice_in_dim(iw, start, Q_BLOCK, axis=1).astype(jnp.float32) * IDX_HEADS ** -0.5
        posq = lax.dynamic_slice_in_dim(positions, start, Q_BLOCK, axis=1)
        q_idx = start + jnp.arange(Q_BLOCK)
        causal = key_idx[None, :] <= q_idx[:, None]
        dots = jnp.einsum('bqhd,bsd->bqhs', iqb, ikf) * IDX_DIM ** -0.5
        score = jnp.einsum('bqh,bqhs->bqs', iwb, jax.nn.relu(dots))
        score = jnp.where(causal[None], score, -jnp.inf)
        _, sel = lax.top_k(score, k_top)
        k_sel = take_rows(k, sel)
        v_sel = take_rows(v, sel)
        bias = rel_table[rel_bucket(posq[:, :, None] - take_rows(positions, sel))]
        logits = jnp.einsum('bqhd,bqkd->bqhk', qb, k_sel).astype(jnp.float32) * A_HEAD_DIM ** -0.5
        logits = logits + jnp.swapaxes(bias, 2, 3).astype(jnp.float32)
        valid = sel <= q_idx[None, :, None]
        logits = jnp.where(valid[:, :, None, :], logits, NEG_INF)
        p = jax.nn.softmax(logits, axis=-1).astype(v.dtype)
        return jnp.einsum('bqhk,bqkd->bqhd', p, v_sel)

    out = lax.map(one_block, jnp.arange(L // Q_BLOCK))
    return jnp.moveaxis(out, 0, 1).reshape(bsz, L, A_WIDTH)


def mla_attention(q_nope, q_rope, k_nope, k_rope, v):
    bsz, L = q_nope.shape[0], q_nope.shape[1]
    key_idx = jnp.arange(L)

    def one_block(bi):
        start = bi * Q_BLOCK
        qn = lax.dynamic_slice_in_dim(q_nope, start, Q_BLOCK, axis=1)
        qr = lax.dynamic_slice_in_dim(q_rope, start, Q_BLOCK, axis=1)
        q_idx = start + jnp.arange(Q_BLOCK)
        logits = (jnp.einsum('bqhd,bshd->bhqs', qn, k_nope)
                  + jnp.einsum('bqhr,bsr->bhqs', qr, k_rope)).astype(jnp.float32) * B_QK_DIM ** -0.5
        causal = key_idx[None, :] <= q_idx[:, None]
        logits = jnp.where(causal[None, None], logits, NEG_INF)
        p = jax.nn.softmax(logits, axis=-1).astype(v.dtype)
        return jnp.einsum('bhqs,bshd->bqhd', p, v)

    out = lax.map(one_block, jnp.arange(L // Q_BLOCK))
    return jnp.moveaxis(out, 0, 1).reshape(bsz, L, B_WIDTH)


def setup_inputs(seed: int = 0) -> dict:
    key = jax.random.key(seed)
    ks = jax.random.split(key, 20)
    f32 = jnp.float32

    def nrm(k, shape, scale):
        return jax.random.normal(k, shape, f32) * scale

    def gain(k, shape):
        return 1.0 + 0.02 * jax.random.normal(k, shape, f32)

    x = jax.random.normal(ks[0], (BATCH, SEQ, D_MODEL), f32)
    offset = jax.random.randint(ks[1], (BATCH, 1), 0, 4096, dtype=jnp.int32)
    positions = offset + jnp.arange(SEQ, dtype=jnp.int32)[None, :]
    return {
        "x": x,
        "positions": positions,
        "attn_norm_g": gain(ks[2], (DEPTH, D_MODEL)),
        "w_in": nrm(ks[3], (DEPTH, D_MODEL, D_IN), D_MODEL ** -0.5),
        "b_gate": nrm(ks[4], (DEPTH, N_BRANCHES * D_MODEL), 0.02),
        "q_latent_norm_g": gain(ks[5], (DEPTH, Q_LORA)),
        "kv_latent_norm_g": gain(ks[6], (DEPTH, KV_LORA)),
        "w_uq": nrm(ks[7], (DEPTH, Q_LORA, B_HEADS * B_QK_DIM), Q_LORA ** -0.5),
        "w_ukv": nrm(ks[8], (DEPTH, KV_LORA, B_HEADS * (B_NOPE_DIM + B_V_DIM)), KV_LORA ** -0.5),
        "w_branch_a": nrm(ks[9], (DEPTH, A_WIDTH, D_MODEL), A_WIDTH ** -0.5),
        "w_branch_b": nrm(ks[10], (DEPTH, B_WIDTH, D_MODEL), B_WIDTH ** -0.5),
        "w_out": nrm(ks[11], (DEPTH, D_MODEL, D_MODEL), D_MODEL ** -0.5),
        "mlp_norm_g": gain(ks[12], (DEPTH, D_MODEL)),
        "w_ff1": nrm(ks[13], (DEPTH, D_MODEL, D_FF), D_MODEL ** -0.5),
        "w_ff2": nrm(ks[14], (DEPTH, D_FF, D_MODEL), D_FF ** -0.5),
        "rel_bias": nrm(ks[15], (REL_BUCKETS, A_HEADS), 0.5),
        "final_norm_g": gain(ks[16], (D_MODEL,)),
    }


def reference(x, positions, attn_norm_g, w_in, b_gate, q_latent_norm_g, kv_latent_norm_g,
              w_uq, w_ukv, w_branch_a, w_branch_b, w_out, mlp_norm_g, w_ff1, w_ff2,
              rel_bias, final_norm_g):
    bsz, L, _ = x.shape
    cos, sin = rope_tables(positions)
    for l in range(DEPTH):
        h = rmsnorm(x, attn_norm_g[l])
        z = h @ w_in[l]
        a_q, a_k, a_v, i_q, i_k, i_w, c_q, c_kv, k_r, g = split_cols(z, IN_SIZES)
        y_a = dsa_sparse_attention(a_q.reshape(bsz, L, A_HEADS, A_HEAD_DIM), a_k, a_v,
                                   i_q.reshape(bsz, L, IDX_HEADS, IDX_DIM), i_k, i_w,
                                   positions, rel_bias)
        qb = (rmsnorm(c_q, q_latent_norm_g[l]) @ w_uq[l]).reshape(bsz, L, B_HEADS, B_QK_DIM)
        q_nope, q_rope = jnp.split(qb, [B_NOPE_DIM], axis=-1)
        q_rope = apply_rope(q_rope, cos[:, :, None], sin[:, :, None])
        kvb = (rmsnorm(c_kv, kv_latent_norm_g[l]) @ w_ukv[l]).reshape(bsz, L, B_HEADS, B_NOPE_DIM + B_V_DIM)
        k_nope, v_b = jnp.split(kvb, [B_NOPE_DIM], axis=-1)
        k_rope = apply_rope(k_r, cos, sin)
        y_b = mla_attention(q_nope, q_rope, k_nope, k_rope, v_b)
        gates = jax.nn.sigmoid((g + b_gate[l]).astype(jnp.float32)).astype(x.dtype)
        gates = gates.reshape(bsz, L, N_BRANCHES, D_MODEL)
        merged = gates[:, :, 0] * (y_a @ w_branch_a[l]) + gates[:, :, 1] * (y_b @ w_branch_b[l])
        x = x + merged @ w_out[l]
        h2 = rmsnorm(x, mlp_norm_g[l])
        x = x + jnp.square(jax.nn.relu(h2 @ w_ff1[l])) @ w_ff2[l]
    return rmsnorm(x, final_norm_g)
```

```python
import math
from contextlib import ExitStack

import numpy as np
import concourse.bass as bass
import concourse.mybir as mybir
from concourse.bass_utils import run_bass_kernel_spmd

F32 = mybir.dt.float32
BF16 = mybir.dt.bfloat16
I32 = mybir.dt.int32
ALU = mybir.AluOpType
AF = mybir.ActivationFunctionType

ENGS = ("pe", "act", "dve", "pool", "sp")

D = 1024
NCORES = 8
EPS = 1e-6
NEG = -30000.0
NIT = 13
LIM = 4.0
NSLOT = 4


class Prog:
    def __init__(self, nc):
        self.nc = nc
        self.ins = {e: [] for e in ENGS}
        self.last_w = {}
        self.readers = {}
        self.dma_count = {}
        self.dma_keys = []

    def _deps(self, eng, reads, writes, is_dma):
        best = {}

        def add(d):
            if d is None:
                return
            k = (d[0], d[1])
            if k not in best or best[k][2] < d[2]:
                best[k] = d

        skip_same = (eng == "pe") and not is_dma
        for r in reads:
            w = self.last_w.get(r)
            if w is not None and not (skip_same and w[0] == "c" and w[1] == eng):
                add(w)
        for r in writes:
            w = self.last_w.get(r)
            if w is not None and not (skip_same and w[0] == "c" and w[1] == eng):
                add(w)
            for rd in self.readers.get(r, ()):
                if not (skip_same and rd[0] == "c" and rd[1] == eng):
                    add(rd)
        return list(best.values())

    def _commit(self, ev, reads, writes):
        for r in reads:
            lst = self.readers.setdefault(r, [])
            for k, o in enumerate(lst):
                if o[0] == ev[0] and o[1] == ev[1]:
                    lst[k] = ev if ev[2] > o[2] else o
                    break
            else:
                lst.append(ev)
        for r in writes:
            self.last_w[r] = ev
            self.readers[r] = []

    def op(self, eng, fn, reads=(), writes=()):
        reads = tuple(reads)
        writes = tuple(writes)
        deps = self._deps(eng, reads, writes, False)
        idx = len(self.ins[eng])
        self.ins[eng].append(dict(fn=fn, deps=deps, dma=None, flag=False))
        self._commit(("c", eng, idx), reads, writes)

    def dma(self, eng, fn, key, reads=(), writes=()):
        reads = tuple(reads)
        writes = tuple(writes)
        deps = self._deps(eng, reads, writes, True)
        if key not in self.dma_count:
            self.dma_count[key] = 0
            self.dma_keys.append(key)
        self.dma_count[key] += 1
        ev = ("d", key, self.dma_count[key])
        self.ins[eng].append(dict(fn=fn, deps=deps, dma=key, flag=False))
        self._commit(ev, reads, writes)

    def emit(self):
        nc = self.nc
        for e in ENGS:
            for rec in self.ins[e]:
                for d in rec["deps"]:
                    if d[0] == "c":
                        self.ins[d[1]][d[2]]["flag"] = True
        semval = {}
        for e in ENGS:
            c = 0
            for i, rec in enumerate(self.ins[e]):
                if rec["flag"]:
                    c += 1
                semval[(e, i)] = c
        with ExitStack() as st:
            esem = {e: st.enter_context(nc.semaphore("s_" + e)) for e in ENGS if e != "sp"}
            dsem = {k: st.enter_context(nc.semaphore("d_%d" % i)) for i, k in enumerate(self.dma_keys)}
            block = st.enter_context(nc.Block())
            total_waits = [0]

            def run(e, eng):
                seen = {}
                for i, rec in enumerate(self.ins[e]):
                    need = {}
                    for d in rec["deps"]:
                        if d[0] == "c":
                            s, v = esem[d[1]], semval[(d[1], d[2])]
                            k = ("c", d[1])
                        else:
                            s, v = dsem[d[1]], 16 * d[2]
                            k = ("d", d[1])
                        if seen.get(k, 0) >= v:
                            continue
                        if k not in need or need[k][1] < v:
                            need[k] = (s, v)
                    for k, (s, v) in need.items():
                        eng.wait_ge(s, v)
                        seen[k] = v
                        total_waits[0] += 1
                    ins = rec["fn"](eng)
                    if rec["dma"] is not None:
                        ins.then_inc(dsem[rec["dma"]], 16)
                    elif rec["flag"]:
                        ins.then_inc(esem[e], 1)
                if e == "sp":
                    for k in self.dma_keys:
                        eng.wait_ge(dsem[k], 16 * self.dma_count[k])

            @block.tensor
            def _(eng):
                run("pe", eng)

            @block.scalar
            def _(eng):
                run("act", eng)

            @block.vector
            def _(eng):
                run("dve", eng)

            @block.gpsimd
            def _(eng):
                run("pool", eng)

            @block.sync
            def _(eng):
                run("sp", eng)
        self.n_instr = {e: len(self.ins[e]) for e in ENGS}
        self.n_waits = total_waits[0]


A_Q, A_K, A_V, I_Q, I_K, I_W, C_Q, C_KV, K_R, GATE = 0, 512, 576, 640, 896, 960, 964, 1220, 1348, 1380
NB1 = 29


def w1_block_cols():
    blocks = []
    for c in range(4):
        blocks.append(np.arange(A_Q + 128 * c, A_Q + 128 * c + 128))
    blocks.append(np.concatenate([np.arange(A_K, A_K + 64)] * 2))
    for c in range(2):
        blocks.append(np.arange(I_Q + 128 * c, I_Q + 128 * c + 128))
    blocks.append(np.concatenate([np.arange(I_K, I_K + 64)] * 2))
    for c in range(2):
        blocks.append(np.arange(C_Q + 128 * c, C_Q + 128 * c + 128))
    blocks.append(np.arange(C_KV, C_KV + 128))
    kr = np.arange(K_R, K_R + 32)
    blocks.append(np.concatenate([kr] * 4))
    krs = np.concatenate([kr[16:], kr[:16]])
    blocks.append(np.concatenate([krs] * 4))
    for c in range(16):
        blocks.append(np.arange(GATE + 128 * c, GATE + 128 * c + 128))
    assert len(blocks) == NB1
    return blocks


def rel_bucket_np(n):
    n = np.maximum(n, 0)
    nf = np.maximum(n.astype(np.float32), np.float32(1.0))
    lb = 16 + (np.log(nf / np.float32(16)) / np.float32(math.log(8.0)) * np.float32(16)).astype(np.int32)
    return np.where(n < 16, n, np.minimum(lb, 31))


class Builder:
    def __init__(self, L, NL, NSEQ):
        self.L, self.NL, self.NSEQ = L, NL, NSEQ
        self.NT = L // 128
        self.NCH = L // 512
        self.KTOP = min(256, L // 4)
        self.gbc = 0
        self.slotc = 0
        self.tmpc = 0
        self.ptc = 0
        self.ptmc = 0
        self.gbufc = 0
        self.pstc = 0

    def mm(self, out, lhsT, rhs, start, stop, R, W):
        self.P.op("pe", lambda e: e.matmul(out, lhsT=lhsT, rhs=rhs, start=start, stop=stop), R, W)

    def tr(self, out, in_, ident, R, W):
        self.P.op("pe", lambda e: e.transpose(out, in_, ident), R, W)

    def act(self, out, in_, func, R, W, bias=None, scale=None):
        kw = {}
        if bias is not None:
            kw["bias"] = bias
        if scale is not None:
            kw["scale"] = scale
        self.P.op("act", lambda e: e.activation(out=out, in_=in_, func=func, **kw), R, W)

    def ts(self, eng, out, in0, s1, s2, op0, op1, R, W, accum=None):
        kw = {}
        if op1 is not None:
            kw["op1"] = op1
        if accum is not None:
            kw["accum_out"] = accum
        self.P.op(eng, lambda e: e.tensor_scalar(out=out, in0=in0, scalar1=s1, scalar2=s2, op0=op0, **kw), R, W)

    def tt(self, eng, out, in0, in1, op, R, W):
        self.P.op(eng, lambda e: e.tensor_tensor(out=out, in0=in0, in1=in1, op=op), R, W)

    def stt(self, out, in0, scalar, in1, op0, op1, R, W):
        self.P.op("dve", lambda e: e.scalar_tensor_tensor(out=out, in0=in0, scalar=scalar, in1=in1, op0=op0, op1=op1), R, W)

    def cp(self, eng, out, in_, R, W):
        if eng == "act":
            self.P.op("act", lambda e: e.activation(out=out, in_=in_, func=AF.Copy), R, W)
        else:
            self.P.op(eng, lambda e: e.tensor_copy(out=out, in_=in_), R, W)

    def memset(self, eng, ap, val, W):
        self.P.op(eng, lambda e: e.memset(ap, val), (), W)

    def dma(self, q, out, in_, key, R, W):
        self.P.dma(q, lambda e: e.dma_start(out=out, in_=in_), key, R, W)

    def gb(self):
        k = self.gbc % 4
        self.gbc += 1
        return self.ps[k], "ps%d" % k

    def tmpf(self):
        k = self.tmpc % len(self.tmp_aps)
        self.tmpc += 1
        return self.tmp_aps[k]

    def load_w(self, src, ncols):
        k = self.slotc % NSLOT
        self.slotc += 1
        self.dma("pool", self.wr[k][:, 0:ncols], src, "w%d" % k, (), ["w%d" % k])
        return self.wr[k], "w%d" % k

    def HR(self, c, nt=None):
        if nt is None:
            return ["H%d_%d" % (c, n) for n in range(self.NCH)]
        return ["H%d_%d" % (c, nt)]

    def UR(self, c, nt=None):
        if nt is None:
            return ["U%d_%d" % (c, n) for n in range(self.NCH)]
        return ["U%d_%d" % (c, nt)]

    def XR(self, c, nt):
        return ["X%d_%d" % (c, nt)]

    def Hv(self, c, a, b):
        return self.H[:, c * self.L + a: c * self.L + b]

    def Uv(self, c, a, b):
        return self.U[:, c * self.L + a: c * self.L + b]

    def rmsnorm_nt(self, srcs, src_res, g_aps, dim, dsts, dst_res):
        n = len(srcs)
        for c in range(n):
            sq, sqr = self.tmpf()
            self.act(sq, srcs[c], AF.Square, src_res[c], [sqr])
            self.mm(self.ps[6][:, :], self.ones_f[:, :], sq, c == 0, c == n - 1, [sqr, "ones"], ["ps6"])
        self.act(self.rstd[:, :], self.ps[6][:, :], AF.Sqrt, ["ps6", "epsb"], ["rstd"], bias=self.epsb[:, 0:1], scale=1.0 / dim)
        self.P.op("dve", lambda e: e.reciprocal(out=self.rstd[:, :], in_=self.rstd[:, :]), ["rstd"], ["rstd"])
        for c in range(n):
            self.stt(dsts[c], srcs[c], g_aps[c], self.rstd[:, :], ALU.mult, ALU.mult, list(src_res[c]) + ["rstd", "gains"], dst_res[c])

    def build(self):
        L, NL, NSEQ, NT, NCH = self.L, self.NL, self.NSEQ, self.NT, self.NCH
        nc = bass.Bass("TRN2", target_bir_lowering=False)
        self.nc = nc

        def din(name, shape, dt=F32):
            return nc.dram_tensor(name, shape, dt, kind="ExternalInput").ap()

        x = din("x", [NSEQ, L, D])
        pos = din("pos", [NSEQ, 1, L], I32)
        gains = din("gains", [128, NL * 35 + 8])
        w1b = din("w1b", [NL, NB1, 128, 1024])
        w1t = din("w1t", [NL, 128, 8 * 68])
        wuq = din("wuq", [NL, 128, 2 * 768])
        wuqs = din("wuqs", [NL, 128, 2 * 768])
        wukv = din("wukv", [NL, 128, 1024])
        wa = din("wa", [NL, 8, 128, 512])
        wb = din("wb", [NL, 8, 128, 512])
        wo = din("wo", [NL, 8, 128, 1024])
        wf1 = din("wf1", [NL, 32, 128, 1024])
        wf2 = din("wf2", [NL, 32, 128, 1024])
        band_d = din("band", [8, 128, 256])
        rb31_d = din("rb31", [1, 8])
        cmats = din("cmats", [5, 128, 128])
        rconst = din("rconst", [128, 4])
        y = nc.dram_tensor("y", [NSEQ, L, D], F32, kind="ExternalOutput").ap()
        gsc = nc.dram_tensor("gsc", [16, 128, L], BF16, kind="Internal").ap()
        lat = nc.dram_tensor("lat", [5, 128, L], BF16, kind="Internal").ap()

        with ExitStack() as st:
            def sb(name, shape, dt):
                return st.enter_context(nc.sbuf_tensor(name, shape, dt))

            def psum(name, shape, dt):
                return st.enter_context(nc.psum_tensor(name, shape, dt))

            XT = sb("XT", [128, 8, L], F32)
            self.H = sb("H", [128, 8 * L], BF16)
            self.U = sb("U", [128, 8 * L], BF16)
            self.wr = [sb("wr%d" % k, [128, 1024], BF16) for k in range(NSLOT)]
            wuq_sb = sb("wuq_sb", [128, 1536], BF16)
            wuqs_sb = sb("wuqs_sb", [128, 1536], BF16)
            wukv_sb = sb("wukv_sb", [128, 1024], BF16)
            w1t_sb = sb("w1t_sb", [128, 8 * 68], BF16)
            va = sb("va", [128, NT, 65], BF16)
            vb = [sb("vb%d" % k, [128, NT, 65], BF16) for k in range(2)]
            iwabs = sb("iwabs", [128, NT, 4], F32)
            iwsgn = sb("iwsgn", [128, NT, 4], F32)
            score = sb("score", [128, max(L, 1024)], F32)
            Mts = sb("Mts", [128, L], BF16)
            MT = [sb("MT%d" % k, [128, L], BF16) for k in range(2)]
            PT = [sb("PT%d" % k, [128, 512], BF16) for k in range(2)]
            PTm = [sb("PTm%d" % k, [128, 512], BF16) for k in range(2)]
            self.tmp = [sb("tmp%d" % k, [128, 512], F32) for k in range(2)]
            self.rstd = sb("rstd", [128, 512], F32)
            ytok = [sb("ytok%d" % k, [128, 512], BF16) for k in range(2)]
            cosT = sb("cosT", [128, L], BF16)
            sinT = sb("sinT", [128, L], BF16)
            gbuf = [sb("gbuf%d" % k, [128, 512], BF16) for k in range(4)]
            band = sb("band_sb", [128, 8, 256], BF16)
            ident_f = sb("ident_f", [128, 128], F32)
            ident_b = sb("ident_b", [128, 128], BF16)
            negtri_b = sb("negtri_b", [128, 128], BF16)
            negtri2 = sb("negtri2", [128, 128], F32)
            tril_b = sb("tril_b", [128, 128], BF16)
            self.ones_f = sb("ones_f", [128, 128], F32)
            self.epsb = sb("epsb", [128, 1], F32)
            gains_sb = sb("gains_sb", [128, NL * 35 + 8], F32)
            rb31 = sb("rb31_sb", [128, 8], F32)
            rb31n = sb("rb31n", [128, 8], F32)
            rconst_sb = sb("rconst_sb", [128, 4], F32)
            thr = sb("thr", [128, 1], F32)
            cnt = sb("cnt", [128, 1], F32)
            uu = sb("uu", [128, 1], F32)
            rec = sb("rec", [128, 8], F32)
            posi = sb("posi", [128, 512], I32)
            ki = sb("ki", [128, 512], I32)
            self.ps = [psum("ps%d" % k, [128, 512], F32) for k in range(7)]
            self.tmp_aps = [(self.tmp[0][:, :], "tmp0"), (self.tmp[1][:, :], "tmp1"),
                            (posi[:, :].bitcast(F32), "posi"), (ki[:, :].bitcast(F32), "ki")]
            pst = psum("pst", [128, 1024], BF16)

            P = Prog(nc)
            self.P = P
            ps = self.ps

            def g_attn(l, c):
                return gains_sb[:, l * 35 + c: l * 35 + c + 1]

            def g_mlp(l, c):
                return gains_sb[:, l * 35 + 8 + c: l * 35 + 9 + c]

            def g_q(l, c):
                return gains_sb[:, l * 35 + 16 + c: l * 35 + 17 + c]

            def g_kv(l):
                return gains_sb[:, l * 35 + 18: l * 35 + 19]

            def b_gate(l, c):
                return gains_sb[:, l * 35 + 19 + c: l * 35 + 20 + c]

            def g_fin(c):
                return gains_sb[:, NL * 35 + c: NL * 35 + c + 1]

            self.dma("sp", gains_sb[:, :], gains[:, :], "c_gains", (), ["gains"])
            self.dma("sp", ident_f[:, :], cmats[0, :, :], "c_identf", (), ["identf"])
            self.dma("pool", ident_b[:, :], cmats[0, :, :], "c_identb", (), ["identb"])
            self.dma("pool", negtri_b[:, :], cmats[1, :, :], "c_negtri", (), ["negtri"])
            self.dma("sp", negtri2[:, :], cmats[2, :, :], "c_negtri2", (), ["negtri2"])
            self.dma("pool", tril_b[:, :], cmats[3, :, :], "c_tril", (), ["tril"])
            self.dma("sp", rconst_sb[:, :], rconst[:, :], "c_rconst", (), ["rconst"])
            self.dma("sp", rb31[:, :], rb31_d.partition_broadcast(128), "c_rb31", (), ["rb31"])
            self.memset("dve", self.ones_f[:, :], 1.0, ["ones"])
            self.memset("dve", self.epsb[:, :], EPS, ["epsb"])
            self.memset("dve", va[:, :, 64:65], 1.0, ["va"])
            self.memset("dve", vb[0][:, :, 64:65], 1.0, ["vb0"])
            self.memset("dve", vb[1][:, :, 64:65], 1.0, ["vb1"])
            self.ts("dve", rb31n[:, :], rb31[:, :], -1.0, None, ALU.mult, None, ["rb31"], ["rb31n"])
            for h in range(8):
                self.dma("sp", self.rstd[:, 0:256], band_d[h, :, :], "c_bandf", (), ["rstd"])
                self.ts("dve", band[:, h, :], self.rstd[:, 0:256], rb31n[:, h:h + 1], None, ALU.add, None, ["rstd", "rb31n"], ["band"])

            for s in range(NSEQ):
                self.seq(s, x, pos, y, XT, gsc, lat, locals())
            P.emit()
            self.stats = (P.n_instr, P.n_waits)
        return nc

    def seq(self, s, x, pos, y, XT, gsc, lat, env):
        L, NL, NT, NCH = self.L, self.NL, self.NT, self.NCH
        ps = self.ps
        P = self.P
        g = env
        score, Mts, MT, PT, PTm = g["score"], g["Mts"], g["MT"], g["PT"], g["PTm"]
        ident_f, ident_b, negtri_b, negtri2, tril_b = g["ident_f"], g["ident_b"], g["negtri_b"], g["negtri2"], g["tril_b"]
        cosT, sinT, rconst_sb, posi, ki = g["cosT"], g["sinT"], g["rconst_sb"], g["posi"], g["ki"]
        pst, va, vb, iwabs, iwsgn = g["pst"], g["va"], g["vb"], g["iwabs"], g["iwsgn"]
        thr, cnt, uu, rec, rb31, band = g["thr"], g["cnt"], g["uu"], g["rec"], g["rb31"], g["band"]
        ytok, gbuf = g["ytok"], g["gbuf"]
        wuq_sb, wuqs_sb, wukv_sb, w1t_sb = g["wuq_sb"], g["wuqs_sb"], g["wukv_sb"], g["w1t_sb"]
        w1b, w1t, wuq, wuqs, wukv, wa, wb, wo, wf1, wf2 = (g[k] for k in ("w1b", "w1t", "wuq", "wuqs", "wukv", "wa", "wb", "wo", "wf1", "wf2"))
        g_attn, g_mlp, g_q, g_kv, b_gate, g_fin = g["g_attn"], g["g_mlp"], g["g_q"], g["g_kv"], g["b_gate"], g["g_fin"]
        KTOP = self.KTOP

        for nt in range(NCH):
            cs = slice(nt * 512, nt * 512 + 512)
            self.dma("sp", posi[:, :], pos[s, :, cs].partition_broadcast(128), "posi", (), ["posi"])
            for tab, c0, name in ((cosT, 0, "cosT"), (sinT, 2, "sinT")):
                t0, r0 = self.tmp[0][:, :], "tmp0"
                self.cp("dve", t0, posi[:, :], ["posi"], [r0])
                t1, r1 = self.tmp[1][:, :], "tmp1"
                self.ts("dve", t1, t0, rconst_sb[:, c0:c0 + 1], rconst_sb[:, c0 + 1:c0 + 2], ALU.mult, ALU.add, [r0, "rconst"], [r1])
                self.cp("dve", ki[:, :], t1, [r1], ["ki"])
                self.cp("dve", t0, ki[:, :], ["ki"], [r0])
                self.tt("dve", t1, t1, t0, ALU.subtract, [r0, r1], [r1])
                self.act(tab[:, cs], t1, AF.Sin, [r1], [name], scale=6.2831845)

        for i in range(NT):
            sg = score[:, 0:1024]
            self.dma("sp", sg, x[s, i * 128:(i + 1) * 128, :], "xin", (), ["score"])
            for half in range(2):
                bank, br = self.gb()
                for c4 in range(4):
                    c = half * 4 + c4
                    self.tr(bank[:, c4 * 128:(c4 + 1) * 128], sg[:, c * 128:(c + 1) * 128], ident_f[:, :], ["score", "identf"], [br])
                dst = XT[:, half * 4:half * 4 + 4, i * 128:(i + 1) * 128]
                src = bank[:, :].rearrange("p (c t) -> p c t", c=4)
                self.cp("act", dst, src, [br], ["X%d_%d" % (c, i // 4) for c in range(half * 4, half * 4 + 4)])

        for l in range(NL):
            self.layer(s, l, env)

        for nt in range(NCH):
            cs = slice(nt * 512, nt * 512 + 512)
            for c in range(8):
                sq, sqr = self.tmpf()
                self.act(sq, XT[:, c, cs], AF.Square, self.XR(c, nt), [sqr])
                self.mm(ps[6][:, :], self.ones_f[:, :], sq, c == 0, c == 7, [sqr, "ones"], ["ps6"])
            self.act(self.rstd[:, :], ps[6][:, :], AF.Sqrt, ["ps6", "epsb"], ["rstd"], bias=self.epsb[:, 0:1], scale=1.0 / D)
            P.op("dve", lambda e: e.reciprocal(out=self.rstd[:, :], in_=self.rstd[:, :]), ["rstd"], ["rstd"])
            for ti in range(4):
                i = nt * 4 + ti
                sg = score[:, 0:1024]
                for half in range(2):
                    bank, br = self.gb()
                    for c4 in range(4):
                        c = half * 4 + c4
                        t0, r0 = self.tmpf()
                        self.stt(t0[:, 0:128], XT[:, c, i * 128:(i + 1) * 128], g_fin(c), self.rstd[:, ti * 128:(ti + 1) * 128],
                                 ALU.mult, ALU.mult, self.XR(c, nt) + ["rstd", "gains"], [r0])
                        self.tr(bank[:, c4 * 128:(c4 + 1) * 128], t0[:, 0:128], ident_f[:, :], [r0, "identf"], [br])
                    self.cp("act", sg[:, half * 512:(half + 1) * 512], bank[:, :], [br], ["score"])
                self.dma("sp", y[s, i * 128:(i + 1) * 128, :], sg, "yout", ["score"], ())

    def layer(self, s, l, env):
        L, NL, NT, NCH = self.L, self.NL, self.NT, self.NCH
        ps = self.ps
        P = self.P
        g = env
        XT = g["XT"]
        score, Mts, MT, PT, PTm = g["score"], g["Mts"], g["MT"], g["PT"], g["PTm"]
        ident_f, ident_b, negtri_b, negtri2, tril_b = g["ident_f"], g["ident_b"], g["negtri_b"], g["negtri2"], g["tril_b"]
        cosT, sinT = g["cosT"], g["sinT"]
        pst, va, vb, iwabs, iwsgn = g["pst"], g["va"], g["vb"], g["iwabs"], g["iwsgn"]
        thr, cnt, uu, rec, rb31, band = g["thr"], g["cnt"], g["uu"], g["rec"], g["rb31"], g["band"]
        ytok, gbuf = g["ytok"], g["gbuf"]
        gsc, lat = g["gsc"], g["lat"]
        wuq_sb, wuqs_sb, wukv_sb, w1t_sb = g["wuq_sb"], g["wuqs_sb"], g["wukv_sb"], g["w1t_sb"]
        w1b, w1t, wuq, wuqs, wukv, wa, wb, wo, wf1, wf2 = (g[k] for k in ("w1b", "w1t", "wuq", "wuqs", "wukv", "wa", "wb", "wo", "wf1", "wf2"))
        g_attn, g_mlp, g_q, g_kv, b_gate = g["g_attn"], g["g_mlp"], g["g_q"], g["g_kv"], g["b_gate"]
        KTOP = self.KTOP
        Hv, Uv, HR, UR, XR = self.Hv, self.Uv, self.HR, self.UR, self.XR

        self.dma("pool", wuq_sb[:, :], wuq[l, :, :], "wuq", (), ["wuq"])
        self.dma("pool", wuqs_sb[:, :], wuqs[l, :, :], "wuqs", (), ["wuqs"])
        self.dma("pool", wukv_sb[:, :], wukv[l, :, :], "wukv", (), ["wukv"])
        self.dma("pool", w1t_sb[:, :], w1t[l, :, :], "w1t", (), ["w1t"])

        for nt in range(NCH):
            cs = (nt * 512, nt * 512 + 512)
            self.rmsnorm_nt([XT[:, c, cs[0]:cs[1]] for c in range(8)], [XR(c, nt) for c in range(8)],
                            [g_attn(l, c) for c in range(8)], D,
                            [Hv(c, *cs) for c in range(8)], [HR(c, nt) for c in range(8)])

        def evac_plain(unit):
            def f(bank, br, nt):
                self.cp("act", Uv(unit, nt * 512, nt * 512 + 512), bank[:, :], [br], UR(unit, nt))
            return f

        def evac_q(unit):
            def f(bank, br, nt):
                self.act(Uv(unit, nt * 512, nt * 512 + 512), bank[:, :], AF.Copy, [br], UR(unit, nt), scale=0.125)
            return f

        def evac_lat(k):
            def f(bank, br, nt):
                gbt = gbuf[self.gbufc % 4]
                gr = "gbuf%d" % (self.gbufc % 4)
                self.gbufc += 1
                self.cp("act", gbt[:, :], bank[:, :], [br], [gr])
                self.dma("sp", lat[k, :, nt * 512:nt * 512 + 512], gbt[:, :], "st_" + gr, [gr], ["lat%d" % k])
            return f

        def evac_gate(c):
            def f(bank, br, nt):
                gbt = gbuf[self.gbufc % 4]
                gr = "gbuf%d" % (self.gbufc % 4)
                self.gbufc += 1
                self.act(gbt[:, :], bank[:, :], AF.Sigmoid, [br, "gains"], [gr], bias=b_gate(l, c))
                self.dma("sp", gsc[c, :, nt * 512:nt * 512 + 512], gbt[:, :], "st_" + gr, [gr], ["gsc%d" % c])
            return f

        evacs = [evac_q(0), evac_q(1), evac_q(2), evac_q(3), evac_plain(6), evac_plain(4), evac_plain(5), evac_plain(7),
                 evac_lat(0), evac_lat(1), evac_lat(2), evac_lat(3), evac_lat(4)] + [evac_gate(c) for c in range(16)]
        for b in range(NB1):
            wt, wres = self.load_w(w1b[l, b, :, :], 1024)
            for nt in range(NCH):
                bank, br = self.gb()
                for kc in range(8):
                    self.mm(bank[:, :], wt[:, kc * 128:(kc + 1) * 128], Hv(kc, nt * 512, nt * 512 + 512), kc == 0, kc == 7,
                            [wres] + HR(kc, nt), [br])
                evacs[b](bank, br, nt)
        for i in range(NT):
            bank, br = self.gb()
            for kc in range(8):
                self.mm(bank[:, 0:68], Hv(kc, i * 128, (i + 1) * 128), w1t_sb[:, kc * 68:(kc + 1) * 68], kc == 0, kc == 7,
                        ["w1t"] + HR(kc, i // 4), [br])
            self.cp("act", va[:, i, 0:64], bank[:, 0:64], [br], ["va"])
            self.act(iwabs[:, i, :], bank[:, 64:68], AF.Abs, [br], ["iw"], scale=0.0625)
            self.act(iwsgn[:, i, :], bank[:, 64:68], AF.Sign, [br], ["iw"])

        def indexer(i):
            S = 128 * (i + 1)
            for kb in range((S + 511) // 512):
                c0, c1 = kb * 512, min(S, kb * 512 + 512)
                w = c1 - c0
                for h in range(4):
                    unit = 4 + h // 2
                    r0, r1 = (h % 2) * 64, (h % 2) * 64 + 64
                    bank, br = self.gb()
                    self.mm(bank[:, 0:w], self.U[r0:r1, unit * L + i * 128: unit * L + (i + 1) * 128],
                            self.U[r0:r1, 7 * L + c0: 7 * L + c1], True, True,
                            UR(unit, i // 4) + [r for k in range(c0 // 512, (c1 + 511) // 512) for r in UR(7, k)], [br])
                    t0, tr0 = self.tmpf()
                    self.act(t0[:, 0:w], bank[:, 0:w], AF.Relu, [br, "iw"], [tr0], scale=iwabs[:, i, h:h + 1])
                    if h == 0:
                        self.ts("dve", score[:, c0:c1], t0[:, 0:w], iwsgn[:, i, h:h + 1], None, ALU.mult, None, [tr0, "iw"], ["score"])
                    else:
                        self.stt(score[:, c0:c1], t0[:, 0:w], iwsgn[:, i, h:h + 1], score[:, c0:c1], ALU.mult, ALU.add,
                                 [tr0, "iw", "score"], ["score"])
            self.tt("dve", score[:, i * 128:(i + 1) * 128], score[:, i * 128:(i + 1) * 128], negtri2[:, :], ALU.add,
                    ["score", "negtri2"], ["score"])

        def bisect(i):
            S = 128 * (i + 1)
            if S <= KTOP:
                if i > 0:
                    self.memset("dve", Mts[:, 0:i * 128], 1.0, ["Mts"])
                self.cp("dve", Mts[:, i * 128:(i + 1) * 128], tril_b[:, :], ["tril"], ["Mts"])
                return
            self.memset("dve", thr[:, :], 0.0, ["thr"])
            for n in range(NIT):
                stp = LIM / (2.0 ** (n + 1))
                self.ts("dve", Mts[:, 0:S], score[:, 0:S], thr[:, 0:1], 0.0, ALU.is_gt, ALU.add, ["score", "thr"], ["Mts", "cnt"], accum=cnt[:, 0:1])
                self.ts("dve", uu[:, :], cnt[:, :], KTOP - 0.5, 2.0 * stp, ALU.is_ge, ALU.mult, ["cnt"], ["uu"])
                self.stt(thr[:, :], thr[:, :], -stp, uu[:, :], ALU.add, ALU.add, ["thr", "uu"], ["thr"])
            self.ts("dve", Mts[:, 0:S], score[:, 0:S], thr[:, 0:1], None, ALU.is_gt, None, ["score", "thr"], ["Mts"])

        def masktrans(i, buf):
            for g0 in range(0, i + 1, 4):
                jn = min(4, i + 1 - g0)
                half = self.pstc % 2
                self.pstc += 1
                pr = "pst"
                for jj in range(jn):
                    j = g0 + jj
                    self.tr(pst[:, half * 512 + jj * 128: half * 512 + (jj + 1) * 128], Mts[:, j * 128:(j + 1) * 128], ident_b[:, :],
                            ["Mts", "identb"], [pr])
                self.cp("act", MT[buf][:, g0 * 128:(g0 + jn) * 128], pst[:, half * 512: half * 512 + jn * 128], [pr], ["MT%d" % buf])

        def attn_items(i, mixer, head, kT, kres, qT, qres, kdim, vt, vres, acc, accres, acol, mtbuf):
            items = []
            for g0 in range(0, i + 1, 4):
                jn = min(4, i + 1 - g0)
                w = jn * 128
                st = {}

                def qk(g0=g0, jn=jn, st=st):
                    bank, br = self.gb()
                    st["bank"], st["br"] = bank, br
                    for jj in range(jn):
                        j = g0 + jj
                        near = (j >= i - 1) if mixer == "A" else (j == i)
                        self.mm(bank[:, jj * 128:(jj + 1) * 128], kT(j), qT(i), True, not near, kres(j) + qres(i), [br])
                        if near:
                            if mixer == "A":
                                rhs = band[:, head, (i - j) * 128:(i - j) * 128 + 128]
                                rr = ["band"]
                            else:
                                rhs = negtri_b[:, :]
                                rr = ["negtri"]
                            self.mm(bank[:, jj * 128:(jj + 1) * 128], ident_b[:, :], rhs, False, True, ["identb"] + rr, [br])

                def mid(g0=g0, w=w, st=st):
                    bank, br = st["bank"], st["br"]
                    pk = self.ptc % 2
                    self.ptc += 1
                    if mixer == "A":
                        self.act(PT[pk][:, 0:w], bank[:, 0:w], AF.Exp, [br, "rb31"], ["PT%d" % pk], bias=rb31[:, head:head + 1])
                        mk = self.ptmc % 2
                        self.ptmc += 1
                        self.tt("pool", PTm[mk][:, 0:w], PT[pk][:, 0:w], MT[mtbuf][:, g0 * 128: g0 * 128 + w], ALU.mult,
                                ["PT%d" % pk, "MT%d" % mtbuf], ["PTm%d" % mk])
                        st["pt"], st["ptr"] = PTm[mk], "PTm%d" % mk
                    else:
                        self.act(PT[pk][:, 0:w], bank[:, 0:w], AF.Exp, [br], ["PT%d" % pk], scale=96.0 ** -0.5)
                        st["pt"], st["ptr"] = PT[pk], "PT%d" % pk

                def pv(g0=g0, jn=jn, st=st):
                    pt, ptr = st["pt"], st["ptr"]
                    for jj in range(jn):
                        j = g0 + jj
                        self.mm(acc[:, acol:acol + 65], pt[:, jj * 128:(jj + 1) * 128], vt[:, j, :], j == 0, j == i, [ptr, vres], [accres])

                items.append(dict(qk=qk, mid=mid, pv=pv, post=None))
            return items

        def run_items(items, PD=2, hook=None):
            n = len(items)
            hk = (3 * n) // 4
            for j in range(min(PD, n)):
                items[j]["qk"]()
            for k, it in enumerate(items):
                if k + PD < n:
                    items[k + PD]["qk"]()
                it["mid"]()
                it["pv"]()
                if it["post"] is not None:
                    it["post"]()
                if hook is not None and k == hk:
                    hook()
                    hook = None
            if hook is not None:
                hook()

        def y_transposes(src_tile, src_res, dst_fn, dst_res):
            half = self.pstc % 2
            self.pstc += 1
            pr = "pst"
            for c in range(4):
                self.tr(pst[:, half * 512 + c * 128: half * 512 + (c + 1) * 128], src_tile[:, c * 128:(c + 1) * 128], ident_b[:, :],
                        [src_res, "identb"], [pr])
            self.cp("act", dst_fn, pst[:, half * 512: half * 512 + 512].rearrange("p (c t) -> p c t", c=4), [pr], dst_res)

        Hy = self.H[:, :].rearrange("p (c t) -> p c t", c=8)
        Uy = self.U[:, :].rearrange("p (c t) -> p c t", c=8)

        def attnA(i, buf, hook=None):
            items = []
            for h in range(8):
                c = h // 2
                r0, r1 = (h % 2) * 64, (h % 2) * 64 + 64
                acc, accres = (ps[4], "ps4") if h < 4 else (ps[5], "ps5")
                items += attn_items(i, "A", h,
                                    lambda j, r0=r0, r1=r1: self.U[r0:r1, 6 * L + j * 128: 6 * L + (j + 1) * 128], lambda j: UR(6, j // 4),
                                    lambda ii, r0=r0, r1=r1, c=c: self.U[r0:r1, c * L + ii * 128: c * L + (ii + 1) * 128],
                                    lambda ii, c=c: UR(c, ii // 4),
                                    64, va, "va", acc, accres, (h % 4) * 65, buf)
            run_items(items, hook=hook)
            yt = ytok[i % 2]
            yr = "ytok%d" % (i % 2)
            for hh, (acc, accres) in enumerate(((ps[4], "ps4"), (ps[5], "ps5"))):
                a3 = acc[:, 0:260].rearrange("p (h d) -> p h d", d=65)
                P.op("dve", lambda e, a3=a3, hh=hh: e.reciprocal(out=rec[:, hh * 4:hh * 4 + 4], in_=a3[:, :, 64]), [accres], ["rec"])
                for h4 in range(4):
                    h = hh * 4 + h4
                    self.act(yt[:, h * 64:(h + 1) * 64], acc[:, h4 * 65:h4 * 65 + 64], AF.Copy, [accres, "rec"], [yr], scale=rec[:, h:h + 1])
            y_transposes(yt, yr, Hy[:, 0:4, i * 128:(i + 1) * 128], [r for c in range(4) for r in HR(c, i // 4)])

        indexer(0)
        bisect(0)
        masktrans(0, 0)
        for i in range(NT):
            if i + 1 < NT:
                indexer(i + 1)
                bisect(i + 1)
            attnA(i, i % 2, (lambda i=i: masktrans(i + 1, (i + 1) % 2)) if i + 1 < NT else None)

        for k, unit in ((0, 0), (1, 1), (2, 2), (3, 3), (4, 4)):
            self.dma("sp", Uv(unit, 0, L), lat[k, :, :], "latld%d" % k, ["lat%d" % k], UR(unit))
        for nt in range(NCH):
            cs = (nt * 512, nt * 512 + 512)
            self.rmsnorm_nt([Uv(0, *cs), Uv(1, *cs)], [UR(0, nt), UR(1, nt)], [g_q(l, 0), g_q(l, 1)], 256,
                            [Uv(5, *cs), Uv(6, *cs)], [UR(5, nt), UR(6, nt)])
            self.rmsnorm_nt([Uv(2, *cs)], [UR(2, nt)], [g_kv(l)], 128, [Uv(7, *cs)], [UR(7, nt)])
            t0, r0 = self.tmpf()
            t1, r1 = self.tmpf()
            R6 = slice(64, 96)
            self.tt("dve", t0[R6, :], self.U[R6, 3 * L + cs[0]: 3 * L + cs[1]], cosT[R6, cs[0]:cs[1]], ALU.mult, UR(3, nt) + ["cosT"], [r0])
            self.tt("dve", t1[R6, :], self.U[R6, 4 * L + cs[0]: 4 * L + cs[1]], sinT[R6, cs[0]:cs[1]], ALU.mult, UR(4, nt) + ["sinT"], [r1])
            self.tt("dve", self.U[R6, 3 * L + cs[0]: 3 * L + cs[1]], t0[R6, :], t1[R6, :], ALU.add, [r0, r1], UR(3, nt))

        Ytb = self.H[:, 4 * L: 8 * L].rearrange("p (i f) -> p i f", f=512)
        def mla_proj(h):
            qb = h % 2
            kb_ = 2 if h % 2 == 0 else 4
            vbuf = vb[h % 2]
            vres = "vb%d" % (h % 2)
            R6 = slice(64, 96)
            for nt in range(NCH):
                cs = (nt * 512, nt * 512 + 512)
                bA, brA = self.gb()
                bB, brB = self.gb()
                for kc in range(2):
                    self.mm(bA[0:96, :], wuq_sb[:, kc * 768 + 96 * h: kc * 768 + 96 * h + 96], Uv(5 + kc, *cs), kc == 0, kc == 1,
                            ["wuq"] + UR(5 + kc, nt), [brA])
                for kc in range(2):
                    self.mm(bB[0:96, :], wuqs_sb[:, kc * 768 + 96 * h: kc * 768 + 96 * h + 96], Uv(5 + kc, *cs), kc == 0, kc == 1,
                            ["wuqs"] + UR(5 + kc, nt), [brB])
                self.cp("act", self.U[0:64, qb * L + cs[0]: qb * L + cs[1]], bA[0:64, :], [brA], UR(qb, nt))
                t0, r0 = self.tmpf()
                t1, r1 = self.tmpf()
                self.tt("dve", t0[R6, :], bA[R6, :], cosT[R6, cs[0]:cs[1]], ALU.mult, [brA, "cosT"], [r0])
                self.tt("dve", t1[R6, :], bB[R6, :], sinT[R6, cs[0]:cs[1]], ALU.mult, [brB, "sinT"], [r1])
                self.tt("dve", self.U[R6, qb * L + cs[0]: qb * L + cs[1]], t0[R6, :], t1[R6, :], ALU.add, [r0, r1], UR(qb, nt))
                bK, brK = self.gb()
                self.mm(bK[0:64, :], wukv_sb[:, 128 * h: 128 * h + 64], Uv(7, *cs), True, True, ["wukv"] + UR(7, nt), [brK])
                self.cp("act", self.U[0:64, kb_ * L + cs[0]: kb_ * L + cs[1]], bK[0:64, :], [brK], UR(kb_, nt))
                self.cp("pool", self.U[R6, kb_ * L + cs[0]: kb_ * L + cs[1]], self.U[R6, 3 * L + cs[0]: 3 * L + cs[1]], UR(3, nt), UR(kb_, nt))
            for i0 in range(0, NT, 4):
                bV, brV = self.gb()
                for ii in range(4):
                    i = i0 + ii
                    self.mm(bV[:, ii * 64:(ii + 1) * 64], Uv(7, i * 128, (i + 1) * 128), wukv_sb[:, 128 * h + 64: 128 * h + 128], True, True,
                            ["wukv"] + UR(7, i // 4), [brV])
                self.cp("act", vbuf[:, i0:i0 + 4, 0:64], bV[:, 0:256].rearrange("p (i d) -> p i d", d=64), [brV], [vres])

        def mla_attn(h):
            qb = h % 2
            kb_ = 2 if h % 2 == 0 else 4
            vbuf = vb[h % 2]
            vres = "vb%d" % (h % 2)
            R6 = slice(64, 96)
            items = []
            for i0 in range(0, NT, 4):
                acc, accres = (ps[4], "ps4") if (i0 // 4) % 2 == 0 else (ps[5], "ps5")
                for ii in range(4):
                    i = i0 + ii
                    items += attn_items(i, "B", h,
                                        lambda j: self.U[0:96, kb_ * L + j * 128: kb_ * L + (j + 1) * 128], lambda j: UR(kb_, j // 4),
                                        lambda q: self.U[0:96, qb * L + q * 128: qb * L + (q + 1) * 128], lambda q: UR(qb, q // 4),
                                        96, vbuf, vres, acc, accres, ii * 65, 0)

                def post(i0=i0, acc=acc, accres=accres):
                    a3 = acc[:, 0:260].rearrange("p (h d) -> p h d", d=65)
                    P.op("dve", lambda e, a3=a3: e.reciprocal(out=rec[:, 0:4], in_=a3[:, :, 64]), [accres], ["rec"])
                    for ii in range(4):
                        i = i0 + ii
                        self.ts("dve", Ytb[:, i, h * 64:(h + 1) * 64], acc[:, ii * 65: ii * 65 + 64], rec[:, ii:ii + 1], None, ALU.mult, None,
                                [accres, "rec"], HR(4 + i // (NT // 4), None))
                items[-1]["post"] = post
            run_items(items)

        mla_proj(0)
        for h in range(8):
            if h + 1 < 8:
                mla_proj(h + 1)
            mla_attn(h)
        for i in range(NT):
            half = self.pstc % 2
            self.pstc += 1
            pr = "pst"
            for c in range(4):
                self.tr(pst[:, half * 512 + c * 128: half * 512 + (c + 1) * 128], Ytb[:, i, c * 128:(c + 1) * 128], ident_b[:, :],
                        HR(4 + i // (NT // 4), None) + ["identb"], [pr])
            self.cp("act", Uy[:, 0:4, i * 128:(i + 1) * 128], pst[:, half * 512: half * 512 + 512].rearrange("p (c t) -> p c t", c=4), [pr],
                    [r for c in range(4) for r in UR(c, i // 4)])

        def merged_view(oc, a, b):
            return Uv(4 + oc, a, b) if oc < 4 else Hv(oc, a, b)

        def merged_res(oc, nt):
            return UR(4 + oc, nt) if oc < 4 else HR(oc, nt)

        for oc in range(8):
            wA, wAr = self.load_w(wa[l, oc, :, :], 512)
            wB, wBr = self.load_w(wb[l, oc, :, :], 512)
            for nt in range(NCH):
                cs = (nt * 512, nt * 512 + 512)
                bA, brA = self.gb()
                for kc in range(4):
                    self.mm(bA[:, :], wA[:, kc * 128:(kc + 1) * 128], Hv(kc, *cs), kc == 0, kc == 3, [wAr] + HR(kc, nt), [brA])
                bB, brB = self.gb()
                for kc in range(4):
                    self.mm(bB[:, :], wB[:, kc * 128:(kc + 1) * 128], Uv(kc, *cs), kc == 0, kc == 3, [wBr] + UR(kc, nt), [brB])
                g0 = gbuf[self.gbufc % 4]
                g0r = "gbuf%d" % (self.gbufc % 4)
                self.gbufc += 1
                g1 = gbuf[self.gbufc % 4]
                g1r = "gbuf%d" % (self.gbufc % 4)
                self.gbufc += 1
                self.dma("sp", g0[:, :], gsc[oc, :, cs[0]:cs[1]], "ld_" + g0r, ["gsc%d" % oc], [g0r])
                self.dma("sp", g1[:, :], gsc[8 + oc, :, cs[0]:cs[1]], "ld_" + g1r, ["gsc%d" % (8 + oc)], [g1r])
                t0, r0 = self.tmpf()
                t1, r1 = self.tmpf()
                self.tt("dve", t0, bA[:, :], g0[:, :], ALU.mult, [brA, g0r], [r0])
                self.tt("dve", t1, bB[:, :], g1[:, :], ALU.mult, [brB, g1r], [r1])
                self.tt("dve", merged_view(oc, *cs), t0, t1, ALU.add, [r0, r1], merged_res(oc, nt))
        for oc in range(8):
            wO, wOr = self.load_w(wo[l, oc, :, :], 1024)
            for nt in range(NCH):
                cs = (nt * 512, nt * 512 + 512)
                bank, br = self.gb()
                for kc in range(8):
                    self.mm(bank[:, :], wO[:, kc * 128:(kc + 1) * 128], merged_view(kc, *cs), kc == 0, kc == 7, [wOr] + merged_res(kc, nt), [br])
                self.tt("dve", XT[:, oc, cs[0]:cs[1]], XT[:, oc, cs[0]:cs[1]], bank[:, :], ALU.add, XR(oc, nt) + [br], XR(oc, nt))

        for nt in range(NCH):
            cs = (nt * 512, nt * 512 + 512)
            self.rmsnorm_nt([XT[:, c, cs[0]:cs[1]] for c in range(8)], [XR(c, nt) for c in range(8)],
                            [g_mlp(l, c) for c in range(8)], D,
                            [Hv(c, *cs) for c in range(8)], [HR(c, nt) for c in range(8)])
        for grp in range(4):
            for hcl in range(8):
                hc = grp * 8 + hcl
                w1, w1r = self.load_w(wf1[l, hc, :, :], 1024)
                for nt in range(NCH):
                    cs = (nt * 512, nt * 512 + 512)
                    bank, br = self.gb()
                    for kc in range(8):
                        self.mm(bank[:, :], w1[:, kc * 128:(kc + 1) * 128], Hv(kc, *cs), kc == 0, kc == 7, [w1r] + HR(kc, nt), [br])
                    t0, r0 = self.tmpf()
                    self.act(t0, bank[:, :], AF.Relu, [br], [r0])
                    self.tt("dve", Uv(hcl, *cs), t0, t0, ALU.mult, [r0], UR(hcl, nt))
            for oc in range(8):
                w2, w2r = self.load_w(wf2[l, grp * 8 + oc, :, :], 1024)
                for nt in range(NCH):
                    cs = (nt * 512, nt * 512 + 512)
                    bank, br = self.gb()
                    for kc in range(8):
                        self.mm(bank[:, :], w2[:, kc * 128:(kc + 1) * 128], Uv(kc, *cs), kc == 0, kc == 7, [w2r] + UR(kc, nt), [br])
                    self.tt("dve", XT[:, oc, cs[0]:cs[1]], XT[:, oc, cs[0]:cs[1]], bank[:, :], ALU.add, XR(oc, nt) + [br], XR(oc, nt))


def prep_weights(inp, NL):
    f = np.float32
    w_in = np.asarray(inp["w_in"], f)
    blocks = w1_block_cols()
    w1b = np.empty((NL, NB1, 128, 8, 128), f)
    for b, cols in enumerate(blocks):
        blk = w_in[:NL][:, :, cols]
        w1b[:, b] = blk.reshape(NL, 8, 128, 128).transpose(0, 2, 1, 3)
    w1b = w1b.reshape(NL, NB1, 128, 1024)
    tcols = np.concatenate([np.arange(A_V, A_V + 64), np.arange(I_W, I_W + 4)])
    w1t = w_in[:NL][:, :, tcols].reshape(NL, 8, 128, 68).transpose(0, 2, 1, 3).reshape(NL, 128, 8 * 68)
    w_uq = np.asarray(inp["w_uq"], f)[:NL]
    swcols = []
    for h in range(8):
        base = 96 * h
        swcols += list(range(base, base + 64)) + list(range(base + 80, base + 96)) + list(range(base + 64, base + 80))
    w_uqs = w_uq[:, :, swcols]
    wuq = w_uq.reshape(NL, 2, 128, 768).transpose(0, 2, 1, 3).reshape(NL, 128, 1536)
    wuqs = w_uqs.reshape(NL, 2, 128, 768).transpose(0, 2, 1, 3).reshape(NL, 128, 1536)
    wukv = np.asarray(inp["w_ukv"], f)[:NL]

    def blk_oc(w, kchunks):
        return w.reshape(NL, kchunks, 128, 8, 128).transpose(0, 3, 2, 1, 4).reshape(NL, 8, 128, kchunks * 128)

    wa = blk_oc(np.asarray(inp["w_branch_a"], f)[:NL], 4)
    wb = blk_oc(np.asarray(inp["w_branch_b"], f)[:NL], 4)
    wo = blk_oc(np.asarray(inp["w_out"], f)[:NL], 8)
    wf1 = np.asarray(inp["w_ff1"], f)[:NL].reshape(NL, 8, 128, 32, 128).transpose(0, 3, 2, 1, 4).reshape(NL, 32, 128, 1024)
    wf2 = np.asarray(inp["w_ff2"], f)[:NL].reshape(NL, 4, 8, 128, 8, 128).transpose(0, 1, 4, 3, 2, 5).reshape(NL, 32, 128, 1024)
    gains = np.empty((128, NL * 35 + 8), f)
    for l in range(NL):
        o = l * 35
        gains[:, o:o + 8] = np.asarray(inp["attn_norm_g"], f)[l].reshape(8, 128).T
        gains[:, o + 8:o + 16] = np.asarray(inp["mlp_norm_g"], f)[l].reshape(8, 128).T
        gains[:, o + 16:o + 18] = np.asarray(inp["q_latent_norm_g"], f)[l].reshape(2, 128).T
        gains[:, o + 18:o + 19] = np.asarray(inp["kv_latent_norm_g"], f)[l].reshape(1, 128).T
        gains[:, o + 19:o + 35] = np.asarray(inp["b_gate"], f)[l].reshape(16, 128).T
    gains[:, NL * 35:] = np.asarray(inp["final_norm_g"], f).reshape(8, 128).T
    rel_bias = np.asarray(inp["rel_bias"], f)
    sp = np.arange(128)[:, None]
    m = np.arange(256)[None, :]
    n = m - sp
    band = rel_bias[rel_bucket_np(n)]
    band = np.where((n >= 0)[:, :, None], band, f(NEG)).astype(f).transpose(2, 0, 1)
    rb31 = rel_bias[31:32, :]
    cm = np.zeros((5, 128, 128), f)
    cm[0] = np.eye(128, dtype=f)
    a = np.arange(128)
    cm[1] = np.where(a[None, :] >= a[:, None], 0.0, NEG)
    cm[2] = np.where(a[None, :] <= a[:, None], 0.0, -1e30)
    cm[3] = np.where(a[None, :] <= a[:, None], 1.0, 0.0)
    inv_freq = (10000.0 ** (-np.arange(0, 32, 2, dtype=np.float64) / 32.0))
    rc = np.zeros((128, 4), f)
    for p in range(128):
        r = p % 32
        rc[p, 0] = inv_freq[r % 16] / (2 * math.pi)
        rc[p, 1] = 0.25
        rc[p, 2] = inv_freq[r % 16] / (2 * math.pi)
        rc[p, 3] = 0.5 if r < 16 else 0.0
    c = np.ascontiguousarray
    return dict(gains=c(gains), w1b=c(w1b), w1t=c(w1t), wuq=c(wuq), wuqs=c(wuqs), wukv=c(wukv), wa=c(wa), wb=c(wb), wo=c(wo),
                wf1=c(wf1), wf2=c(wf2), band=c(band), rb31=c(rb31), cmats=c(cm), rconst=c(rc))


_CACHE = {}


def run(inp, L, NL, NSEQ, ncores):
    key = (L, NL, NSEQ)
    if key not in _CACHE:
        b = Builder(L, NL, NSEQ)
        _CACHE[key] = (b.build(), b)
    nc, b = _CACHE[key]
    shared = prep_weights(inp, NL)
    x = np.asarray(inp["x"], np.float32)
    pos = np.asarray(inp["positions"], np.int32)
    in_maps = []
    for c in range(ncores):
        m = dict(shared)
        m["x"] = np.ascontiguousarray(x[c * NSEQ:(c + 1) * NSEQ])
        m["pos"] = np.ascontiguousarray(pos[c * NSEQ:(c + 1) * NSEQ].reshape(NSEQ, 1, L))
        in_maps.append(m)
    res = run_bass_kernel_spmd(nc, in_maps, core_ids=list(range(ncores)))
    return np.concatenate([r["y"] for r in res.results], axis=0)


def kernel(**inputs):
    return run(inputs, 2048, 4, 2, NCORES).astype(np.float32)
```

```python
import math
from contextlib import ExitStack

import numpy as np
import concourse.bass as bass
import concourse.mybir as mybir
from concourse.bass_utils import run_bass_kernel_spmd

F32 = mybir.dt.float32
BF16 = mybir.dt.bfloat16
I32 = mybir.dt.int32
ALU = mybir.AluOpType
AF = mybir.ActivationFunctionType

ENGS = ("pe", "act", "dve", "pool", "sp")

D = 1024
NCORES = 8
EPS = 1e-6
NEG = -30000.0
NIT = 13
LIM = 4.0
NSLOT = 4


class Prog:
    def __init__(self, nc):
        self.nc = nc
        self.ins = {e: [] for e in ENGS}
        self.last_w = {}
        self.readers = {}
        self.dma_count = {}
        self.dma_keys = []

    def _deps(self, eng, reads, writes, is_dma):
        best = {}

        def add(d):
            if d is None:
                return
            k = (d[0], d[1])
            if k not in best or best[k][2] < d[2]:
                best[k] = d

        skip_same = (eng == "pe") and not is_dma
        for r in reads:
            w = self.last_w.get(r)
            if w is not None and not (skip_same and w[0] == "c" and w[1] == eng):
                add(w)
        for r in writes:
            w = self.last_w.get(r)
            if w is not None and not (skip_same and w[0] == "c" and w[1] == eng):
                add(w)
            for rd in self.readers.get(r, ()):
                if not (skip_same and rd[0] == "c" and rd[1] == eng):
                    add(rd)
        return list(best.values())

    def _commit(self, ev, reads, writes):
        for r in reads:
            lst = self.readers.setdefault(r, [])
            for k, o in enumerate(lst):
                if o[0] == ev[0] and o[1] == ev[1]:
                    lst[k] = ev if ev[2] > o[2] else o
                    break
            else:
                lst.append(ev)
        for r in writes:
            self.last_w[r] = ev
            self.readers[r] = []

    def op(self, eng, fn, reads=(), writes=()):
        reads = tuple(reads)
        writes = tuple(writes)
        deps = self._deps(eng, reads, writes, False)
        idx = len(self.ins[eng])
        self.ins[eng].append(dict(fn=fn, deps=deps, dma=None, flag=False))
        self._commit(("c", eng, idx), reads, writes)

    def dma(self, eng, fn, key, reads=(), writes=()):
        reads = tuple(reads)
        writes = tuple(writes)
        deps = self._deps(eng, reads, writes, True)
        if key not in self.dma_count:
            self.dma_count[key] = 0
            self.dma_keys.append(key)
        self.dma_count[key] += 1
        ev = ("d", key, self.dma_count[key])
        self.ins[eng].append(dict(fn=fn, deps=deps, dma=key, flag=False))
        self._commit(ev, reads, writes)

    def emit(self):
        nc = self.nc
        for e in ENGS:
            for rec in self.ins[e]:
                for d in rec["deps"]:
                    if d[0] == "c":
                        self.ins[d[1]][d[2]]["flag"] = True
        semval = {}
        for e in ENGS:
            c = 0
            for i, rec in enumerate(self.ins[e]):
                if rec["flag"]:
                    c += 1
                semval[(e, i)] = c
        with ExitStack() as st:
            esem = {e: st.enter_context(nc.semaphore("s_" + e)) for e in ENGS if e != "sp"}
            dsem = {k: st.enter_context(nc.semaphore("d_%d" % i)) for i, k in enumerate(self.dma_keys)}
            block = st.enter_context(nc.Block())
            total_waits = [0]

            def run(e, eng):
                seen = {}
                for i, rec in enumerate(self.ins[e]):
                    need = {}
                    for d in rec["deps"]:
                        if d[0] == "c":
                            s, v = esem[d[1]], semval[(d[1], d[2])]
                            k = ("c", d[1])
                        else:
                            s, v = dsem[d[1]], 16 * d[2]
                            k = ("d", d[1])
                        if seen.get(k, 0) >= v:
                            continue
                        if k not in need or need[k][1] < v:
                            need[k] = (s, v)
                    for k, (s, v) in need.items():
                        eng.wait_ge(s, v)
                        seen[k] = v
                        total_waits[0] += 1
                    ins = rec["fn"](eng)
                    if rec["dma"] is not None:
                        ins.then_inc(dsem[rec["dma"]], 16)
                    elif rec["flag"]:
                        ins.then_inc(esem[e], 1)
                if e == "sp":
                    for k in self.dma_keys:
                        eng.wait_ge(dsem[k], 16 * self.dma_count[k])

            @block.tensor
            def _(eng):
                run("pe", eng)

            @block.scalar
            def _(eng):
                run("act", eng)

            @block.vector
            def _(eng):
                run("dve", eng)

            @block.gpsimd
            def _(eng):
                run("pool", eng)

            @block.sync
            def _(eng):
                run("sp", eng)
        self.n_instr = {e: len(self.ins[e]) for e in ENGS}
        self.n_waits = total_waits[0]


A_Q, A_K, A_V, I_Q, I_K, I_W, C_Q, C_KV, K_R, GATE = 0, 512, 576, 640, 896, 960, 964, 1220, 1348, 1380
NB1 = 29


def w1_block_cols():
    blocks = []
    for c in range(4):
        blocks.append(np.arange(A_Q + 128 * c, A_Q + 128 * c + 128))
    blocks.append(np.concatenate([np.arange(A_K, A_K + 64)] * 2))
    for c in range(2):
        blocks.append(np.arange(I_Q + 128 * c, I_Q + 128 * c + 128))
    blocks.append(np.concatenate([np.arange(I_K, I_K + 64)] * 2))
    for c in range(2):
        blocks.append(np.arange(C_Q + 128 * c, C_Q + 128 * c + 128))
    blocks.append(np.arange(C_KV, C_KV + 128))
    kr = np.arange(K_R, K_R + 32)
    blocks.append(np.concatenate([kr] * 4))
    krs = np.concatenate([kr[16:], kr[:16]])
    blocks.append(np.concatenate([krs] * 4))
    for c in range(16):
        blocks.append(np.arange(GATE + 128 * c, GATE + 128 * c + 128))
    assert len(blocks) == NB1
    return blocks


def rel_bucket_np(n):
    n = np.maximum(n, 0)
    nf = np.maximum(n.astype(np.float32), np.float32(1.0))
    lb = 16 + (np.log(nf / np.float32(16)) / np.float32(math.log(8.0)) * np.float32(16)).astype(np.int32)
    return np.where(n < 16, n, np.minimum(lb, 31))


class Builder:
    def __init__(self, L, NL, NSEQ):
        self.L, self.NL, self.NSEQ = L, NL, NSEQ
        self.NT = L // 128
        self.NCH = L // 512
        self.KTOP = min(256, L // 4)
        self.gbc = 0
        self.slotc = 0
        self.tmpc = 0
        self.ptc = 0
        self.ptmc = 0
        self.gbufc = 0
        self.pstc = 0

    def mm(self, out, lhsT, rhs, start, stop, R, W):
        self.P.op("pe", lambda e: e.matmul(out, lhsT=lhsT, rhs=rhs, start=start, stop=stop), R, W)

    def tr(self, out, in_, ident, R, W):
        self.P.op("pe", lambda e: e.transpose(out, in_, ident), R, W)

    def act(self, out, in_, func, R, W, bias=None, scale=None):
        kw = {}
        if bias is not None:
            kw["bias"] = bias
        if scale is not None:
            kw["scale"] = scale
        self.P.op("act", lambda e: e.activation(out=out, in_=in_, func=func, **kw), R, W)

    def ts(self, eng, out, in0, s1, s2, op0, op1, R, W, accum=None):
        kw = {}
        if op1 is not None:
            kw["op1"] = op1
        if accum is not None:
            kw["accum_out"] = accum
        self.P.op(eng, lambda e: e.tensor_scalar(out=out, in0=in0, scalar1=s1, scalar2=s2, op0=op0, **kw), R, W)

    def tt(self, eng, out, in0, in1, op, R, W):
        self.P.op(eng, lambda e: e.tensor_tensor(out=out, in0=in0, in1=in1, op=op), R, W)

    def stt(self, out, in0, scalar, in1, op0, op1, R, W):
        self.P.op("dve", lambda e: e.scalar_tensor_tensor(out=out, in0=in0, scalar=scalar, in1=in1, op0=op0, op1=op1), R, W)

    def cp(self, eng, out, in_, R, W):
        if eng == "act":
            self.P.op("act", lambda e: e.activation(out=out, in_=in_, func=AF.Copy), R, W)
        else:
            self.P.op(eng, lambda e: e.tensor_copy(out=out, in_=in_), R, W)

    def memset(self, eng, ap, val, W):
        self.P.op(eng, lambda e: e.memset(ap, val), (), W)

    def dma(self, q, out, in_, key, R, W):
        self.P.dma(q, lambda e: e.dma_start(out=out, in_=in_), key, R, W)

    def gb(self):
        k = self.gbc % 4
        self.gbc += 1
        return self.ps[k], "ps%d" % k

    def tmpf(self):
        k = self.tmpc % len(self.tmp_aps)
        self.tmpc += 1
        return self.tmp_aps[k]

    def load_w(self, src, ncols):
        k = self.slotc % NSLOT
        self.slotc += 1
        self.dma("pool", self.wr[k][:, 0:ncols], src, "w%d" % k, (), ["w%d" % k])
        return self.wr[k], "w%d" % k

    def HR(self, c, nt=None):
        if nt is None:
            return ["H%d_%d" % (c, n) for n in range(self.NCH)]
        return ["H%d_%d" % (c, nt)]

    def UR(self, c, nt=None):
        if nt is None:
            return ["U%d_%d" % (c, n) for n in range(self.NCH)]
        return ["U%d_%d" % (c, nt)]

    def XR(self, c, nt):
        return ["X%d_%d" % (c, nt)]

    def Hv(self, c, a, b):
        return self.H[:, c * self.L + a: c * self.L + b]

    def Uv(self, c, a, b):
        return self.U[:, c * self.L + a: c * self.L + b]

    def rmsnorm_nt(self, srcs, src_res, g_aps, dim, dsts, dst_res):
        n = len(srcs)
        for c in range(n):
            sq, sqr = self.tmpf()
            self.act(sq, srcs[c], AF.Square, src_res[c], [sqr])
            self.mm(self.ps[6][:, :], self.ones_f[:, :], sq, c == 0, c == n - 1, [sqr, "ones"], ["ps6"])
        self.act(self.rstd[:, :], self.ps[6][:, :], AF.Sqrt, ["ps6", "epsb"], ["rstd"], bias=self.epsb[:, 0:1], scale=1.0 / dim)
        self.P.op("dve", lambda e: e.reciprocal(out=self.rstd[:, :], in_=self.rstd[:, :]), ["rstd"], ["rstd"])
        for c in range(n):
            self.stt(dsts[c], srcs[c], g_aps[c], self.rstd[:, :], ALU.mult, ALU.mult, list(src_res[c]) + ["rstd", "gains"], dst_res[c])

    def build(self):
        L, NL, NSEQ, NT, NCH = self.L, self.NL, self.NSEQ, self.NT, self.NCH
        nc = bass.Bass("TRN2", target_bir_lowering=False)
        self.nc = nc

        def din(name, shape, dt=F32):
            return nc.dram_tensor(name, shape, dt, kind="ExternalInput").ap()

        x = din("x", [NSEQ, L, D])
        pos = din("pos", [NSEQ, 1, L], I32)
        gains = din("gains", [128, NL * 35 + 8])
        w1b = din("w1b", [NL, NB1, 128, 1024])
        w1t = din("w1t", [NL, 128, 8 * 68])
        wuq = din("wuq", [NL, 128, 2 * 768])
        wuqs = din("wuqs", [NL, 128, 2 * 768])
        wukv = din("wukv", [NL, 128, 1024])
        wa = din("wa", [NL, 8, 128, 512])
        wb = din("wb", [NL, 8, 128, 512])
        wo = din("wo", [NL, 8, 128, 1024])
        wf1 = din("wf1", [NL, 32, 128, 1024])
        wf2 = din("wf2", [NL, 32, 128, 1024])
        band_d = din("band", [8, 128, 256])
        rb31_d = din("rb31", [1, 8])
        cmats = din("cmats", [5, 128, 128])
        rconst = din("rconst", [128, 4])
        y = nc.dram_tensor("y", [NSEQ, L, D], F32, kind="ExternalOutput").ap()
        gsc = nc.dram_tensor("gsc", [16, 128, L], BF16, kind="Internal").ap()
        lat = nc.dram_tensor("lat", [5, 128, L], BF16, kind="Internal").ap()

        with ExitStack() as st:
            def sb(name, shape, dt):
                return st.enter_context(nc.sbuf_tensor(name, shape, dt))

            def psum(name, shape, dt):
                return st.enter_context(nc.psum_tensor(name, shape, dt))

            XT = sb("XT", [128, 8, L], F32)
            self.H = sb("H", [128, 8 * L], BF16)
            self.U = sb("U", [128, 8 * L], BF16)
            self.wr = [sb("wr%d" % k, [128, 1024], BF16) for k in range(NSLOT)]
            wuq_sb = sb("wuq_sb", [128, 1536], BF16)
            wuqs_sb = sb("wuqs_sb", [128, 1536], BF16)
            wukv_sb = sb("wukv_sb", [128, 1024], BF16)
            w1t_sb = sb("w1t_sb", [128, 8 * 68], BF16)
            va = sb("va", [128, NT, 65], BF16)
            vb = [sb("vb%d" % k, [128, NT, 65], BF16) for k in range(2)]
            iwabs = sb("iwabs", [128, NT, 4], F32)
            iwsgn = sb("iwsgn", [128, NT, 4], F32)
            score = sb("score", [128, max(L, 1024)], F32)
            Mts = sb("Mts", [128, L], BF16)
            MT = [sb("MT%d" % k, [128, L], BF16) for k in range(2)]
            PT = [sb("PT%d" % k, [128, 512], BF16) for k in range(2)]
            PTm = [sb("PTm%d" % k, [128, 512], BF16) for k in range(2)]
            self.tmp = [sb("tmp%d" % k, [128, 512], F32) for k in range(2)]
            self.rstd = sb("rstd", [128, 512], F32)
            ytok = [sb("ytok%d" % k, [128, 512], BF16) for k in range(2)]
            cosT = sb("cosT", [128, L], BF16)
            sinT = sb("sinT", [128, L], BF16)
            gbuf = [sb("gbuf%d" % k, [128, 512], BF16) for k in range(4)]
            band = sb("band_sb", [128, 8, 256], BF16)
            ident_f = sb("ident_f", [128, 128], F32)
            ident_b = sb("ident_b", [128, 128], BF16)
            negtri_b = sb("negtri_b", [128, 128], BF16)
            negtri2 = sb("negtri2", [128, 128], F32)
            tril_b = sb("tril_b", [128, 128], BF16)
            self.ones_f = sb("ones_f", [128, 128], F32)
            self.epsb = sb("epsb", [128, 1], F32)
            gains_sb = sb("gains_sb", [128, NL * 35 + 8], F32)
            rb31 = sb("rb31_sb", [128, 8], F32)
            rb31n = sb("rb31n", [128, 8], F32)
            rconst_sb = sb("rconst_sb", [128, 4], F32)
            thr = sb("thr", [128, 1], F32)
            cnt = sb("cnt", [128, 1], F32)
            uu = sb("uu", [128, 1], F32)
            rec = sb("rec", [128, 8], F32)
            posi = sb("posi", [128, 512], I32)
            ki = sb("ki", [128, 512], I32)
            self.ps = [psum("ps%d" % k, [128, 512], F32) for k in range(7)]
            self.tmp_aps = [(self.tmp[0][:, :], "tmp0"), (self.tmp[1][:, :], "tmp1"),
                            (posi[:, :].bitcast(F32), "posi"), (ki[:, :].bitcast(F32), "ki")]
            pst = psum("pst", [128, 1024], BF16)

            P = Prog(nc)
            self.P = P
            ps = self.ps

            def g_attn(l, c):
                return gains_sb[:, l * 35 + c: l * 35 + c + 1]

            def g_mlp(l, c):
                return gains_sb[:, l * 35 + 8 + c: l * 35 + 9 + c]

            def g_q(l, c):
                return gains_sb[:, l * 35 + 16 + c: l * 35 + 17 + c]

            def g_kv(l):
                return gains_sb[:, l * 35 + 18: l * 35 + 19]

            def b_gate(l, c):
                return gains_sb[:, l * 35 + 19 + c: l * 35 + 20 + c]

            def g_fin(c):
                return gains_sb[:, NL * 35 + c: NL * 35 + c + 1]

            self.dma("sp", gains_sb[:, :], gains[:, :], "c_gains", (), ["gains"])
            self.dma("sp", ident_f[:, :], cmats[0, :, :], "c_identf", (), ["identf"])
            self.dma("pool", ident_b[:, :], cmats[0, :, :], "c_identb", (), ["identb"])
            self.dma("pool", negtri_b[:, :], cmats[1, :, :], "c_negtri", (), ["negtri"])
            self.dma("sp", negtri2[:, :], cmats[2, :, :], "c_negtri2", (), ["negtri2"])
            self.dma("pool", tril_b[:, :], cmats[3, :, :], "c_tril", (), ["tril"])
            self.dma("sp", rconst_sb[:, :], rconst[:, :], "c_rconst", (), ["rconst"])
            self.dma("sp", rb31[:, :], rb31_d.partition_broadcast(128), "c_rb31", (), ["rb31"])
            self.memset("dve", self.ones_f[:, :], 1.0, ["ones"])
            self.memset("dve", self.epsb[:, :], EPS, ["epsb"])
            self.memset("dve", va[:, :, 64:65], 1.0, ["va"])
            self.memset("dve", vb[0][:, :, 64:65], 1.0, ["vb0"])
            self.memset("dve", vb[1][:, :, 64:65], 1.0, ["vb1"])
            self.ts("dve", rb31n[:, :], rb31[:, :], -1.0, None, ALU.mult, None, ["rb31"], ["rb31n"])
            for h in range(8):
                self.dma("sp", self.rstd[:, 0:256], band_d[h, :, :], "c_bandf", (), ["rstd"])
                self.ts("dve", band[:, h, :], self.rstd[:, 0:256], rb31n[:, h:h + 1], None, ALU.add, None, ["rstd", "rb31n"], ["band"])

            for s in range(NSEQ):
                self.seq(s, x, pos, y, XT, gsc, lat, locals())
            P.emit()
            self.stats = (P.n_instr, P.n_waits)
        return nc

    def seq(self, s, x, pos, y, XT, gsc, lat, env):
        L, NL, NT, NCH = self.L, self.NL, self.NT, self.NCH
        ps = self.ps
        P = self.P
        g = env
        score, Mts, MT, PT, PTm = g["score"], g["Mts"], g["MT"], g["PT"], g["PTm"]
        ident_f, ident_b, negtri_b, negtri2, tril_b = g["ident_f"], g["ident_b"], g["negtri_b"], g["negtri2"], g["tril_b"]
        cosT, sinT, rconst_sb, posi, ki = g["cosT"], g["sinT"], g["rconst_sb"], g["posi"], g["ki"]
        pst, va, vb, iwabs, iwsgn = g["pst"], g["va"], g["vb"], g["iwabs"], g["iwsgn"]
        thr, cnt, uu, rec, rb31, band = g["thr"], g["cnt"], g["uu"], g["rec"], g["rb31"], g["band"]
        ytok, gbuf = g["ytok"], g["gbuf"]
        wuq_sb, wuqs_sb, wukv_sb, w1t_sb = g["wuq_sb"], g["wuqs_sb"], g["wukv_sb"], g["w1t_sb"]
        w1b, w1t, wuq, wuqs, wukv, wa, wb, wo, wf1, wf2 = (g[k] for k in ("w1b", "w1t", "wuq", "wuqs", "wukv", "wa", "wb", "wo", "wf1", "wf2"))
        g_attn, g_mlp, g_q, g_kv, b_gate, g_fin = g["g_attn"], g["g_mlp"], g["g_q"], g["g_kv"], g["b_gate"], g["g_fin"]
        KTOP = self.KTOP

        for nt in range(NCH):
            cs = slice(nt * 512, nt * 512 + 512)
            self.dma("sp", posi[:, :], pos[s, :, cs].partition_broadcast(128), "posi", (), ["posi"])
            for tab, c0, name in ((cosT, 0, "cosT"), (sinT, 2, "sinT")):
                t0, r0 = self.tmp[0][:, :], "tmp0"
                self.cp("dve", t0, posi[:, :], ["posi"], [r0])
                t1, r1 = self.tmp[1][:, :], "tmp1"
                self.ts("dve", t1, t0, rconst_sb[:, c0:c0 + 1], rconst_sb[:, c0 + 1:c0 + 2], ALU.mult, ALU.add, [r0, "rconst"], [r1])
                self.cp("dve", ki[:, :], t1, [r1], ["ki"])
                self.cp("dve", t0, ki[:, :], ["ki"], [r0])
                self.tt("dve", t1, t1, t0, ALU.subtract, [r0, r1], [r1])
                self.act(tab[:, cs], t1, AF.Sin, [r1], [name], scale=6.2831845)

        for i in range(NT):
            sg = score[:, 0:1024]
            self.dma("sp", sg, x[s, i * 128:(i + 1) * 128, :], "xin", (), ["score"])
            for half in range(2):
                bank, br = self.gb()
                for c4 in range(4):
                    c = half * 4 + c4
                    self.tr(bank[:, c4 * 128:(c4 + 1) * 128], sg[:, c * 128:(c + 1) * 128], ident_f[:, :], ["score", "identf"], [br])
                dst = XT[:, half * 4:half * 4 + 4, i * 128:(i + 1) * 128]
                src = bank[:, :].rearrange("p (c t) -> p c t", c=4)
                self.cp("act", dst, src, [br], ["X%d_%d" % (c, i // 4) for c in range(half * 4, half * 4 + 4)])

        for l in range(NL):
            self.layer(s, l, env)

        for nt in range(NCH):
            cs = slice(nt * 512, nt * 512 + 512)
            for c in range(8):
                sq, sqr = self.tmpf()
                self.act(sq, XT[:, c, cs], AF.Square, self.XR(c, nt), [sqr])
                self.mm(ps[6][:, :], self.ones_f[:, :], sq, c == 0, c == 7, [sqr, "ones"], ["ps6"])
            self.act(self.rstd[:, :], ps[6][:, :], AF.Sqrt, ["ps6", "epsb"], ["rstd"], bias=self.epsb[:, 0:1], scale=1.0 / D)
            P.op("dve", lambda e: e.reciprocal(out=self.rstd[:, :], in_=self.rstd[:, :]), ["rstd"], ["rstd"])
            for ti in range(4):
                i = nt * 4 + ti
                sg = score[:, 0:1024]
                for half in range(2):
                    bank, br = self.gb()
                    for c4 in range(4):
                        c = half * 4 + c4
                        t0, r0 = self.tmpf()
                        self.stt(t0[:, 0:128], XT[:, c, i * 128:(i + 1) * 128], g_fin(c), self.rstd[:, ti * 128:(ti + 1) * 128],
                                 ALU.mult, ALU.mult, self.XR(c, nt) + ["rstd", "gains"], [r0])
                        self.tr(bank[:, c4 * 128:(c4 + 1) * 128], t0[:, 0:128], ident_f[:, :], [r0, "identf"], [br])
                    self.cp("act", sg[:, half * 512:(half + 1) * 512], bank[:, :], [br], ["score"])
                self.dma("sp", y[s, i * 128:(i + 1) * 128, :], sg, "yout", ["score"], ())

    def layer(self, s, l, env):
        L, NL, NT, NCH = self.L, self.NL, self.NT, self.NCH
        ps = self.ps
        P = self.P
        g = env
        XT = g["XT"]
        score, Mts, MT, PT, PTm = g["score"], g["Mts"], g["MT"], g["PT"], g["PTm"]
        ident_f, ident_b, negtri_b, negtri2, tril_b = g["ident_f"], g["ident_b"], g["negtri_b"], g["negtri2"], g["tril_b"]
        cosT, sinT = g["cosT"], g["sinT"]
        pst, va, vb, iwabs, iwsgn = g["pst"], g["va"], g["vb"], g["iwabs"], g["iwsgn"]
        thr, cnt, uu, rec, rb31, band = g["thr"], g["cnt"], g["uu"], g["rec"], g["rb31"], g["band"]
        ytok, gbuf = g["ytok"], g["gbuf"]
        gsc, lat = g["gsc"], g["lat"]
        wuq_sb, wuqs_sb, wukv_sb, w1t_sb = g["wuq_sb"], g["wuqs_sb"], g["wukv_sb"], g["w1t_sb"]
        w1b, w1t, wuq, wuqs, wukv, wa, wb, wo, wf1, wf2 = (g[k] for k in ("w1b", "w1t", "wuq", "wuqs", "wukv", "wa", "wb", "wo", "wf1", "wf2"))
        g_attn, g_mlp, g_q, g_kv, b_gate = g["g_attn"], g["g_mlp"], g["g_q"], g["g_kv"], g["b_gate"]
        KTOP = self.KTOP
        Hv, Uv, HR, UR, XR = self.Hv, self.Uv, self.HR, self.UR, self.XR

        self.dma("pool", wuq_sb[:, :], wuq[l, :, :], "wuq", (), ["wuq"])
        self.dma("pool", wuqs_sb[:, :], wuqs[l, :, :], "wuqs", (), ["wuqs"])
        self.dma("pool", wukv_sb[:, :], wukv[l, :, :], "wukv", (), ["wukv"])
        self.dma("pool", w1t_sb[:, :], w1t[l, :, :], "w1t", (), ["w1t"])

        for nt in range(NCH):
            cs = (nt * 512, nt * 512 + 512)
            self.rmsnorm_nt([XT[:, c, cs[0]:cs[1]] for c in range(8)], [XR(c, nt) for c in range(8)],
                            [g_attn(l, c) for c in range(8)], D,
                            [Hv(c, *cs) for c in range(8)], [HR(c, nt) for c in range(8)])

        def evac_plain(unit):
            def f(bank, br, nt):
                self.cp("act", Uv(unit, nt * 512, nt * 512 + 512), bank[:, :], [br], UR(unit, nt))
            return f

        def evac_q(unit):
            def f(bank, br, nt):
                self.act(Uv(unit, nt * 512, nt * 512 + 512), bank[:, :], AF.Copy, [br], UR(unit, nt), scale=0.125)
            return f

        def evac_lat(k):
            def f(bank, br, nt):
                gbt = gbuf[self.gbufc % 4]
                gr = "gbuf%d" % (self.gbufc % 4)
                self.gbufc += 1
                self.cp("act", gbt[:, :], bank[:, :], [br], [gr])
                self.dma("sp", lat[k, :, nt * 512:nt * 512 + 512], gbt[:, :], "st_" + gr, [gr], ["lat%d" % k])
            return f

        def evac_gate(c):
            def f(bank, br, nt):
                gbt = gbuf[self.gbufc % 4]
                gr = "gbuf%d" % (self.gbufc % 4)
                self.gbufc += 1
                self.act(gbt[:, :], bank[:, :], AF.Sigmoid, [br, "gains"], [gr], bias=b_gate(l, c))
                self.dma("sp", gsc[c, :, nt * 512:nt * 512 + 512], gbt[:, :], "st_" + gr, [gr], ["gsc%d" % c])
            return f

        evacs = [evac_q(0), evac_q(1), evac_q(2), evac_q(3), evac_plain(6), evac_plain(4), evac_plain(5), evac_plain(7),
                 evac_lat(0), evac_lat(1), evac_lat(2), evac_lat(3), evac_lat(4)] + [evac_gate(c) for c in range(16)]
        for b in range(NB1):
            wt, wres = self.load_w(w1b[l, b, :, :], 1024)
            for nt in range(NCH):
                bank, br = self.gb()
                for kc in range(8):
                    self.mm(bank[:, :], wt[:, kc * 128:(kc + 1) * 128], Hv(kc, nt * 512, nt * 512 + 512), kc == 0, kc == 7,
                            [wres] + HR(kc, nt), [br])
                evacs[b](bank, br, nt)
        for i in range(NT):
            bank, br = self.gb()
            for kc in range(8):
                self.mm(bank[:, 0:68], Hv(kc, i * 128, (i + 1) * 128), w1t_sb[:, kc * 68:(kc + 1) * 68], kc == 0, kc == 7,
                        ["w1t"] + HR(kc, i // 4), [br])
            self.cp("act", va[:, i, 0:64], bank[:, 0:64], [br], ["va"])
            self.act(iwabs[:, i, :], bank[:, 64:68], AF.Abs, [br], ["iw"], scale=0.0625)
            self.act(iwsgn[:, i, :], bank[:, 64:68], AF.Sign, [br], ["iw"])

        def indexer(i):
            S = 128 * (i + 1)
            for kb in range((S + 511) // 512):
                c0, c1 = kb * 512, min(S, kb * 512 + 512)
                w = c1 - c0
                for h in range(4):
                    unit = 4 + h // 2
                    r0, r1 = (h % 2) * 64, (h % 2) * 64 + 64
                    bank, br = self.gb()
                    self.mm(bank[:, 0:w], self.U[r0:r1, unit * L + i * 128: unit * L + (i + 1) * 128],
                            self.U[r0:r1, 7 * L + c0: 7 * L + c1], True, True,
                            UR(unit, i // 4) + [r for k in range(c0 // 512, (c1 + 511) // 512) for r in UR(7, k)], [br])
                    t0, tr0 = self.tmpf()
                    self.act(t0[:, 0:w], bank[:, 0:w], AF.Relu, [br, "iw"], [tr0], scale=iwabs[:, i, h:h + 1])
                    if h == 0:
                        self.ts("dve", score[:, c0:c1], t0[:, 0:w], iwsgn[:, i, h:h + 1], None, ALU.mult, None, [tr0, "iw"], ["score"])
                    else:
                        self.stt(score[:, c0:c1], t0[:, 0:w], iwsgn[:, i, h:h + 1], score[:, c0:c1], ALU.mult, ALU.add,
                                 [tr0, "iw", "score"], ["score"])
            self.tt("dve", score[:, i * 128:(i + 1) * 128], score[:, i * 128:(i + 1) * 128], negtri2[:, :], ALU.add,
                    ["score", "negtri2"], ["score"])

        def bisect(i):
            S = 128 * (i + 1)
            if S <= KTOP:
                if i > 0:
                    self.memset("dve", Mts[:, 0:i * 128], 1.0, ["Mts"])
                self.cp("dve", Mts[:, i * 128:(i + 1) * 128], tril_b[:, :], ["tril"], ["Mts"])
                return
            self.memset("dve", thr[:, :], 0.0, ["thr"])
            for n in range(NIT):
                stp = LIM / (2.0 ** (n + 1))
                self.ts("dve", Mts[:, 0:S], score[:, 0:S], thr[:, 0:1], 0.0, ALU.is_gt, ALU.add, ["score", "thr"], ["Mts", "cnt"], accum=cnt[:, 0:1])
                self.ts("dve", uu[:, :], cnt[:, :], KTOP - 0.5, 2.0 * stp, ALU.is_ge, ALU.mult, ["cnt"], ["uu"])
                self.stt(thr[:, :], thr[:, :], -stp, uu[:, :], ALU.add, ALU.add, ["thr", "uu"], ["thr"])
            self.ts("dve", Mts[:, 0:S], score[:, 0:S], thr[:, 0:1], None, ALU.is_gt, None, ["score", "thr"], ["Mts"])

        def masktrans(i, buf):
            for g0 in range(0, i + 1, 4):
                jn = min(4, i + 1 - g0)
                half = self.pstc % 2
                self.pstc += 1
                pr = "pst"
                for jj in range(jn):
                    j = g0 + jj
                    self.tr(pst[:, half * 512 + jj * 128: half * 512 + (jj + 1) * 128], Mts[:, j * 128:(j + 1) * 128], ident_b[:, :],
                            ["Mts", "identb"], [pr])
                self.cp("act", MT[buf][:, g0 * 128:(g0 + jn) * 128], pst[:, half * 512: half * 512 + jn * 128], [pr], ["MT%d" % buf])

        def attn_items(i, mixer, head, kT, kres, qT, qres, kdim, vt, vres, acc, accres, acol, mtbuf):
            items = []
            for g0 in range(0, i + 1, 4):
                jn = min(4, i + 1 - g0)
                w = jn * 128
                st = {}

                def qk(g0=g0, jn=jn, st=st):
                    bank, br = self.gb()
                    st["bank"], st["br"] = bank, br
                    for jj in range(jn):
                        j = g0 + jj
                        near = (j >= i - 1) if mixer == "A" else (j == i)
                        self.mm(bank[:, jj * 128:(jj + 1) * 128], kT(j), qT(i), True, not near, kres(j) + qres(i), [br])
                        if near:
                            if mixer == "A":
                                rhs = band[:, head, (i - j) * 128:(i - j) * 128 + 128]
                                rr = ["band"]
                            else:
                                rhs = negtri_b[:, :]
                                rr = ["negtri"]
                            self.mm(bank[:, jj * 128:(jj + 1) * 128], ident_b[:, :], rhs, False, True, ["identb"] + rr, [br])

                def mid(g0=g0, w=w, st=st):
                    bank, br = st["bank"], st["br"]
                    pk = self.ptc % 2
                    self.ptc += 1
                    if mixer == "A":
                        self.act(PT[pk][:, 0:w], bank[:, 0:w], AF.Exp, [br, "rb31"], ["PT%d" % pk], bias=rb31[:, head:head + 1])
                        mk = self.ptmc % 2
                        self.ptmc += 1
                        self.tt("pool", PTm[mk][:, 0:w], PT[pk][:, 0:w], MT[mtbuf][:, g0 * 128: g0 * 128 + w], ALU.mult,
                                ["PT%d" % pk, "MT%d" % mtbuf], ["PTm%d" % mk])
                        st["pt"], st["ptr"] = PTm[mk], "PTm%d" % mk
                    else:
                        self.act(PT[pk][:, 0:w], bank[:, 0:w], AF.Exp, [br], ["PT%d" % pk], scale=96.0 ** -0.5)
                        st["pt"], st["ptr"] = PT[pk], "PT%d" % pk

                def pv(g0=g0, jn=jn, st=st):
                    pt, ptr = st["pt"], st["ptr"]
                    for jj in range(jn):
                        j = g0 + jj
                        self.mm(acc[:, acol:acol + 65], pt[:, jj * 128:(jj + 1) * 128], vt[:, j, :], j == 0, j == i, [ptr, vres], [accres])

                items.append(dict(qk=qk, mid=mid, pv=pv, post=None))
            return items

        def run_items(items, PD=3, hook=None):
            n = len(items)
            hk = (3 * n) // 4
            for j in range(min(PD, n)):
                items[j]["qk"]()
            for k, it in enumerate(items):
                if k + PD < n:
                    items[k + PD]["qk"]()
                it["mid"]()
                it["pv"]()
                if it["post"] is not None:
                    it["post"]()
                if hook is not None and k == hk:
                    hook()
                    hook = None
            if hook is not None:
                hook()

        def y_transposes(src_tile, src_res, dst_fn, dst_res):
            half = self.pstc % 2
            self.pstc += 1
            pr = "pst"
            for c in range(4):
                self.tr(pst[:, half * 512 + c * 128: half * 512 + (c + 1) * 128], src_tile[:, c * 128:(c + 1) * 128], ident_b[:, :],
                        [src_res, "identb"], [pr])
            self.cp("act", dst_fn, pst[:, half * 512: half * 512 + 512].rearrange("p (c t) -> p c t", c=4), [pr], dst_res)

        Hy = self.H[:, :].rearrange("p (c t) -> p c t", c=8)
        Uy = self.U[:, :].rearrange("p (c t) -> p c t", c=8)

        def attnA(i, buf, hook=None):
            items = []
            for h in range(8):
                c = h // 2
                r0, r1 = (h % 2) * 64, (h % 2) * 64 + 64
                acc, accres = (ps[4], "ps4") if h < 4 else (ps[5], "ps5")
                items += attn_items(i, "A", h,
                                    lambda j, r0=r0, r1=r1: self.U[r0:r1, 6 * L + j * 128: 6 * L + (j + 1) * 128], lambda j: UR(6, j // 4),
                                    lambda ii, r0=r0, r1=r1, c=c: self.U[r0:r1, c * L + ii * 128: c * L + (ii + 1) * 128],
                                    lambda ii, c=c: UR(c, ii // 4),
                                    64, va, "va", acc, accres, (h % 4) * 65, buf)
            run_items(items, hook=hook)
            yt = ytok[i % 2]
            yr = "ytok%d" % (i % 2)
            for hh, (acc, accres) in enumerate(((ps[4], "ps4"), (ps[5], "ps5"))):
                a3 = acc[:, 0:260].rearrange("p (h d) -> p h d", d=65)
                P.op("dve", lambda e, a3=a3, hh=hh: e.reciprocal(out=rec[:, hh * 4:hh * 4 + 4], in_=a3[:, :, 64]), [accres], ["rec"])
                for h4 in range(4):
                    h = hh * 4 + h4
                    self.act(yt[:, h * 64:(h + 1) * 64], acc[:, h4 * 65:h4 * 65 + 64], AF.Copy, [accres, "rec"], [yr], scale=rec[:, h:h + 1])
            y_transposes(yt, yr, Hy[:, 0:4, i * 128:(i + 1) * 128], [r for c in range(4) for r in HR(c, i // 4)])

        indexer(0)
        bisect(0)
        masktrans(0, 0)
        for i in range(NT):
            if i + 1 < NT:
                indexer(i + 1)
                bisect(i + 1)
            attnA(i, i % 2, (lambda i=i: masktrans(i + 1, (i + 1) % 2)) if i + 1 < NT else None)

        for k, unit in ((0, 0), (1, 1), (2, 2), (3, 3), (4, 4)):
            self.dma("sp", Uv(unit, 0, L), lat[k, :, :], "latld%d" % k, ["lat%d" % k], UR(unit))
        for nt in range(NCH):
            cs = (nt * 512, nt * 512 + 512)
            self.rmsnorm_nt([Uv(0, *cs), Uv(1, *cs)], [UR(0, nt), UR(1, nt)], [g_q(l, 0), g_q(l, 1)], 256,
                            [Uv(5, *cs), Uv(6, *cs)], [UR(5, nt), UR(6, nt)])
            self.rmsnorm_nt([Uv(2, *cs)], [UR(2, nt)], [g_kv(l)], 128, [Uv(7, *cs)], [UR(7, nt)])
            t0, r0 = self.tmpf()
            t1, r1 = self.tmpf()
            R6 = slice(64, 96)
            self.tt("dve", t0[R6, :], self.U[R6, 3 * L + cs[0]: 3 * L + cs[1]], cosT[R6, cs[0]:cs[1]], ALU.mult, UR(3, nt) + ["cosT"], [r0])
            self.tt("dve", t1[R6, :], self.U[R6, 4 * L + cs[0]: 4 * L + cs[1]], sinT[R6, cs[0]:cs[1]], ALU.mult, UR(4, nt) + ["sinT"], [r1])
            self.tt("dve", self.U[R6, 3 * L + cs[0]: 3 * L + cs[1]], t0[R6, :], t1[R6, :], ALU.add, [r0, r1], UR(3, nt))

        Ytb = self.H[:, 4 * L: 8 * L].rearrange("p (i f) -> p i f", f=512)
        def mla_proj(h):
            qb = h % 2
            kb_ = 2 if h % 2 == 0 else 4
            vbuf = vb[h % 2]
            vres = "vb%d" % (h % 2)
            R6 = slice(64, 96)
            for nt in range(NCH):
                cs = (nt * 512, nt * 512 + 512)
                bA, brA = self.gb()
                bB, brB = self.gb()
                for kc in range(2):
                    self.mm(bA[0:96, :], wuq_sb[:, kc * 768 + 96 * h: kc * 768 + 96 * h + 96], Uv(5 + kc, *cs), kc == 0, kc == 1,
                            ["wuq"] + UR(5 + kc, nt), [brA])
                for kc in range(2):
                    self.mm(bB[0:96, :], wuqs_sb[:, kc * 768 + 96 * h: kc * 768 + 96 * h + 96], Uv(5 + kc, *cs), kc == 0, kc == 1,
                            ["wuqs"] + UR(5 + kc, nt), [brB])
                self.cp("act", self.U[0:64, qb * L + cs[0]: qb * L + cs[1]], bA[0:64, :], [brA], UR(qb, nt))
                t0, r0 = self.tmpf()
                t1, r1 = self.tmpf()
                self.tt("dve", t0[R6, :], bA[R6, :], cosT[R6, cs[0]:cs[1]], ALU.mult, [brA, "cosT"], [r0])
                self.tt("dve", t1[R6, :], bB[R6, :], sinT[R6, cs[0]:cs[1]], ALU.mult, [brB, "sinT"], [r1])
                self.tt("dve", self.U[R6, qb * L + cs[0]: qb * L + cs[1]], t0[R6, :], t1[R6, :], ALU.add, [r0, r1], UR(qb, nt))
                bK, brK = self.gb()
                self.mm(bK[0:64, :], wukv_sb[:, 128 * h: 128 * h + 64], Uv(7, *cs), True, True, ["wukv"] + UR(7, nt), [brK])
                self.cp("act", self.U[0:64, kb_ * L + cs[0]: kb_ * L + cs[1]], bK[0:64, :], [brK], UR(kb_, nt))
                self.cp("pool", self.U[R6, kb_ * L + cs[0]: kb_ * L + cs[1]], self.U[R6, 3 * L + cs[0]: 3 * L + cs[1]], UR(3, nt), UR(kb_, nt))
            for i0 in range(0, NT, 4):
                bV, brV = self.gb()
                for ii in range(4):
                    i = i0 + ii
                    self.mm(bV[:, ii * 64:(ii + 1) * 64], Uv(7, i * 128, (i + 1) * 128), wukv_sb[:, 128 * h + 64: 128 * h + 128], True, True,
                            ["wukv"] + UR(7, i // 4), [brV])
                self.cp("act", vbuf[:, i0:i0 + 4, 0:64], bV[:, 0:256].rearrange("p (i d) -> p i d", d=64), [brV], [vres])

        def mla_attn(h):
            qb = h % 2
            kb_ = 2 if h % 2 == 0 else 4
            vbuf = vb[h % 2]
            vres = "vb%d" % (h % 2)
            R6 = slice(64, 96)
            items = []
            for i0 in range(0, NT, 4):
                acc, accres = (ps[4], "ps4") if (i0 // 4) % 2 == 0 else (ps[5], "ps5")
                for ii in range(4):
                    i = i0 + ii
                    items += attn_items(i, "B", h,
                                        lambda j: self.U[0:96, kb_ * L + j * 128: kb_ * L + (j + 1) * 128], lambda j: UR(kb_, j // 4),
                                        lambda q: self.U[0:96, qb * L + q * 128: qb * L + (q + 1) * 128], lambda q: UR(qb, q // 4),
                                        96, vbuf, vres, acc, accres, ii * 65, 0)

                def post(i0=i0, acc=acc, accres=accres):
                    a3 = acc[:, 0:260].rearrange("p (h d) -> p h d", d=65)
                    P.op("dve", lambda e, a3=a3: e.reciprocal(out=rec[:, 0:4], in_=a3[:, :, 64]), [accres], ["rec"])
                    for ii in range(4):
                        i = i0 + ii
                        self.ts("dve", Ytb[:, i, h * 64:(h + 1) * 64], acc[:, ii * 65: ii * 65 + 64], rec[:, ii:ii + 1], None, ALU.mult, None,
                                [accres, "rec"], HR(4 + i // (NT // 4), None))
                items[-1]["post"] = post
            run_items(items)

        mla_proj(0)
        for h in range(8):
            if h + 1 < 8:
                mla_proj(h + 1)
            mla_attn(h)
        for i in range(NT):
            half = self.pstc % 2
            self.pstc += 1
            pr = "pst"
            for c in range(4):
                self.tr(pst[:, half * 512 + c * 128: half * 512 + (c + 1) * 128], Ytb[:, i, c * 128:(c + 1) * 128], ident_b[:, :],
                        HR(4 + i // (NT // 4), None) + ["identb"], [pr])
            self.cp("act", Uy[:, 0:4, i * 128:(i + 1) * 128], pst[:, half * 512: half * 512 + 512].rearrange("p (c t) -> p c t", c=4), [pr],
                    [r for c in range(4) for r in UR(c, i // 4)])

        def merged_view(oc, a, b):
            return Uv(4 + oc, a, b) if oc < 4 else Hv(oc, a, b)

        def merged_res(oc, nt):
            return UR(4 + oc, nt) if oc < 4 else HR(oc, nt)

        for oc in range(8):
            wA, wAr = self.load_w(wa[l, oc, :, :], 512)
            wB, wBr = self.load_w(wb[l, oc, :, :], 512)
            for nt in range(NCH):
                cs = (nt * 512, nt * 512 + 512)
                bA, brA = self.gb()
                for kc in range(4):
                    self.mm(bA[:, :], wA[:, kc * 128:(kc + 1) * 128], Hv(kc, *cs), kc == 0, kc == 3, [wAr] + HR(kc, nt), [brA])
                bB, brB = self.gb()
                for kc in range(4):
                    self.mm(bB[:, :], wB[:, kc * 128:(kc + 1) * 128], Uv(kc, *cs), kc == 0, kc == 3, [wBr] + UR(kc, nt), [brB])
                g0 = gbuf[self.gbufc % 4]
                g0r = "gbuf%d" % (self.gbufc % 4)
                self.gbufc += 1
                g1 = gbuf[self.gbufc % 4]
                g1r = "gbuf%d" % (self.gbufc % 4)
                self.gbufc += 1
                self.dma("sp", g0[:, :], gsc[oc, :, cs[0]:cs[1]], "ld_" + g0r, ["gsc%d" % oc], [g0r])
                self.dma("sp", g1[:, :], gsc[8 + oc, :, cs[0]:cs[1]], "ld_" + g1r, ["gsc%d" % (8 + oc)], [g1r])
                t0, r0 = self.tmpf()
                t1, r1 = self.tmpf()
                self.tt("dve", t0, bA[:, :], g0[:, :], ALU.mult, [brA, g0r], [r0])
                self.tt("dve", t1, bB[:, :], g1[:, :], ALU.mult, [brB, g1r], [r1])
                self.tt("dve", merged_view(oc, *cs), t0, t1, ALU.add, [r0, r1], merged_res(oc, nt))
        for oc in range(8):
            wO, wOr = self.load_w(wo[l, oc, :, :], 1024)
            for nt in range(NCH):
                cs = (nt * 512, nt * 512 + 512)
                bank, br = self.gb()
                for kc in range(8):
                    self.mm(bank[:, :], wO[:, kc * 128:(kc + 1) * 128], merged_view(kc, *cs), kc == 0, kc == 7, [wOr] + merged_res(kc, nt), [br])
                self.tt("dve", XT[:, oc, cs[0]:cs[1]], XT[:, oc, cs[0]:cs[1]], bank[:, :], ALU.add, XR(oc, nt) + [br], XR(oc, nt))

        for nt in range(NCH):
            cs = (nt * 512, nt * 512 + 512)
            self.rmsnorm_nt([XT[:, c, cs[0]:cs[1]] for c in range(8)], [XR(c, nt) for c in range(8)],
                            [g_mlp(l, c) for c in range(8)], D,
                            [Hv(c, *cs) for c in range(8)], [HR(c, nt) for c in range(8)])
        for grp in range(4):
            for hcl in range(8):
                hc = grp * 8 + hcl
                w1, w1r = self.load_w(wf1[l, hc, :, :], 1024)
                for nt in range(NCH):
                    cs = (nt * 512, nt * 512 + 512)
                    bank, br = self.gb()
                    for kc in range(8):
                        self.mm(bank[:, :], w1[:, kc * 128:(kc + 1) * 128], Hv(kc, *cs), kc == 0, kc == 7, [w1r] + HR(kc, nt), [br])
                    t0, r0 = self.tmpf()
                    self.act(t0, bank[:, :], AF.Relu, [br], [r0])
                    self.tt("dve", Uv(hcl, *cs), t0, t0, ALU.mult, [r0], UR(hcl, nt))
            for oc in range(8):
                w2, w2r = self.load_w(wf2[l, grp * 8 + oc, :, :], 1024)
                for nt in range(NCH):
                    cs = (nt * 512, nt * 512 + 512)
                    bank, br = self.gb()
                    for kc in range(8):
                        self.mm(bank[:, :], w2[:, kc * 128:(kc + 1) * 128], Uv(kc, *cs), kc == 0, kc == 7, [w2r] + UR(kc, nt), [br])
                    self.tt("dve", XT[:, oc, cs[0]:cs[1]], XT[:, oc, cs[0]:cs[1]], bank[:, :], ALU.add, XR(oc, nt) + [br], XR(oc, nt))


def prep_weights(inp, NL):
    f = np.float32
    w_in = np.asarray(inp["w_in"], f)
    blocks = w1_block_cols()
    w1b = np.empty((NL, NB1, 128, 8, 128), f)
    for b, cols in enumerate(blocks):
        blk = w_in[:NL][:, :, cols]
        w1b[:, b] = blk.reshape(NL, 8, 128, 128).transpose(0, 2, 1, 3)
    w1b = w1b.reshape(NL, NB1, 128, 1024)
    tcols = np.concatenate([np.arange(A_V, A_V + 64), np.arange(I_W, I_W + 4)])
    w1t = w_in[:NL][:, :, tcols].reshape(NL, 8, 128, 68).transpose(0, 2, 1, 3).reshape(NL, 128, 8 * 68)
    w_uq = np.asarray(inp["w_uq"], f)[:NL]
    swcols = []
    for h in range(8):
        base = 96 * h
        swcols += list(range(base, base + 64)) + list(range(base + 80, base + 96)) + list(range(base + 64, base + 80))
    w_uqs = w_uq[:, :, swcols]
    wuq = w_uq.reshape(NL, 2, 128, 768).transpose(0, 2, 1, 3).reshape(NL, 128, 1536)
    wuqs = w_uqs.reshape(NL, 2, 128, 768).transpose(0, 2, 1, 3).reshape(NL, 128, 1536)
    wukv = np.asarray(inp["w_ukv"], f)[:NL]

    def blk_oc(w, kchunks):
        return w.reshape(NL, kchunks, 128, 8, 128).transpose(0, 3, 2, 1, 4).reshape(NL, 8, 128, kchunks * 128)

    wa = blk_oc(np.asarray(inp["w_branch_a"], f)[:NL], 4)
    wb = blk_oc(np.asarray(inp["w_branch_b"], f)[:NL], 4)
    wo = blk_oc(np.asarray(inp["w_out"], f)[:NL], 8)
    wf1 = np.asarray(inp["w_ff1"], f)[:NL].reshape(NL, 8, 128, 32, 128).transpose(0, 3, 2, 1, 4).reshape(NL, 32, 128, 1024)
    wf2 = np.asarray(inp["w_ff2"], f)[:NL].reshape(NL, 4, 8, 128, 8, 128).transpose(0, 1, 4, 3, 2, 5).reshape(NL, 32, 128, 1024)
    gains = np.empty((128, NL * 35 + 8), f)
    for l in range(NL):
        o = l * 35
        gains[:, o:o + 8] = np.asarray(inp["attn_norm_g"], f)[l].reshape(8, 128).T
        gains[:, o + 8:o + 16] = np.asarray(inp["mlp_norm_g"], f)[l].reshape(8, 128).T
        gains[:, o + 16:o + 18] = np.asarray(inp["q_latent_norm_g"], f)[l].reshape(2, 128).T
        gains[:, o + 18:o + 19] = np.asarray(inp["kv_latent_norm_g"], f)[l].reshape(1, 128).T
        gains[:, o + 19:o + 35] = np.asarray(inp["b_gate"], f)[l].reshape(16, 128).T
    gains[:, NL * 35:] = np.asarray(inp["final_norm_g"], f).reshape(8, 128).T
    rel_bias = np.asarray(inp["rel_bias"], f)
    sp = np.arange(128)[:, None]
    m = np.arange(256)[None, :]
    n = m - sp
    band = rel_bias[rel_bucket_np(n)]
    band = np.where((n >= 0)[:, :, None], band, f(NEG)).astype(f).transpose(2, 0, 1)
    rb31 = rel_bias[31:32, :]
    cm = np.zeros((5, 128, 128), f)
    cm[0] = np.eye(128, dtype=f)
    a = np.arange(128)
    cm[1] = np.where(a[None, :] >= a[:, None], 0.0, NEG)
    cm[2] = np.where(a[None, :] <= a[:, None], 0.0, -1e30)
    cm[3] = np.where(a[None, :] <= a[:, None], 1.0, 0.0)
    inv_freq = (10000.0 ** (-np.arange(0, 32, 2, dtype=np.float64) / 32.0))
    rc = np.zeros((128, 4), f)
    for p in range(128):
        r = p % 32
        rc[p, 0] = inv_freq[r % 16] / (2 * math.pi)
        rc[p, 1] = 0.25
        rc[p, 2] = inv_freq[r % 16] / (2 * math.pi)
        rc[p, 3] = 0.5 if r < 16 else 0.0
    c = np.ascontiguousarray
    return dict(gains=c(gains), w1b=c(w1b), w1t=c(w1t), wuq=c(wuq), wuqs=c(wuqs), wukv=c(wukv), wa=c(wa), wb=c(wb), wo=c(wo),
                wf1=c(wf1), wf2=c(wf2), band=c(band), rb31=c(rb31), cmats=c(cm), rconst=c(rc))


_CACHE = {}


def run(inp, L, NL, NSEQ, ncores):
    key = (L, NL, NSEQ)
    if key not in _CACHE:
        b = Builder(L, NL, NSEQ)
        _CACHE[key] = (b.build(), b)
    nc, b = _CACHE[key]
    shared = prep_weights(inp, NL)
    x = np.asarray(inp["x"], np.float32)
    pos = np.asarray(inp["positions"], np.int32)
    in_maps = []
    for c in range(ncores):
        m = dict(shared)
        m["x"] = np.ascontiguousarray(x[c * NSEQ:(c + 1) * NSEQ])
        m["pos"] = np.ascontiguousarray(pos[c * NSEQ:(c + 1) * NSEQ].reshape(NSEQ, 1, L))
        in_maps.append(m)
    res = run_bass_kernel_spmd(nc, in_maps, core_ids=list(range(ncores)))
    return np.concatenate([r["y"] for r in res.results], axis=0)


def kernel(**inputs):
    return run(inputs, 2048, 4, 2, NCORES).astype(np.float32)
```

```python
import math
from contextlib import ExitStack

import numpy as np
import concourse.bass as bass
import concourse.mybir as mybir
from concourse.bass_utils import run_bass_kernel_spmd

F32 = mybir.dt.float32
BF16 = mybir.dt.bfloat16
I32 = mybir.dt.int32
ALU = mybir.AluOpType
AF = mybir.ActivationFunctionType

ENGS = ("pe", "act", "dve", "pool", "sp")

D = 1024
NCORES = 8
EPS = 1e-6
NEG = -30000.0
NIT = 13
LIM = 4.0
NSLOT = 4


class Prog:
    def __init__(self, nc):
        self.nc = nc
        self.ins = {e: [] for e in ENGS}
        self.last_w = {}
        self.readers = {}
        self.dma_count = {}
        self.dma_keys = []

    def _deps(self, eng, reads, writes, is_dma):
        best = {}

        def add(d):
            if d is None:
                return
            k = (d[0], d[1])
            if k not in best or best[k][2] < d[2]:
                best[k] = d

        skip_same = (eng == "pe") and not is_dma
        for r in reads:
            w = self.last_w.get(r)
            if w is not None and not (skip_same and w[0] == "c" and w[1] == eng):
                add(w)
        for r in writes:
            w = self.last_w.get(r)
            if w is not None and not (skip_same and w[0] == "c" and w[1] == eng):
                add(w)
            for rd in self.readers.get(r, ()):
                if not (skip_same and rd[0] == "c" and rd[1] == eng):
                    add(rd)
        return list(best.values())

    def _commit(self, ev, reads, writes):
        for r in reads:
            lst = self.readers.setdefault(r, [])
            for k, o in enumerate(lst):
                if o[0] == ev[0] and o[1] == ev[1]:
                    lst[k] = ev if ev[2] > o[2] else o
                    break
            else:
                lst.append(ev)
        for r in writes:
            self.last_w[r] = ev
            self.readers[r] = []

    def op(self, eng, fn, reads=(), writes=()):
        reads = tuple(reads)
        writes = tuple(writes)
        deps = self._deps(eng, reads, writes, False)
        idx = len(self.ins[eng])
        self.ins[eng].append(dict(fn=fn, deps=deps, dma=None, flag=False))
        self._commit(("c", eng, idx), reads, writes)

    def dma(self, eng, fn, key, reads=(), writes=()):
        reads = tuple(reads)
        writes = tuple(writes)
        deps = self._deps(eng, reads, writes, True)
        if key not in self.dma_count:
            self.dma_count[key] = 0
            self.dma_keys.append(key)
        self.dma_count[key] += 1
        ev = ("d", key, self.dma_count[key])
        self.ins[eng].append(dict(fn=fn, deps=deps, dma=key, flag=False))
        self._commit(ev, reads, writes)

    def emit(self):
        nc = self.nc
        for e in ENGS:
            for rec in self.ins[e]:
                for d in rec["deps"]:
                    if d[0] == "c":
                        self.ins[d[1]][d[2]]["flag"] = True
        semval = {}
        for e in ENGS:
            c = 0
            for i, rec in enumerate(self.ins[e]):
                if rec["flag"]:
                    c += 1
                semval[(e, i)] = c
        with ExitStack() as st:
            esem = {e: st.enter_context(nc.semaphore("s_" + e)) for e in ENGS if e != "sp"}
            dsem = {k: st.enter_context(nc.semaphore("d_%d" % i)) for i, k in enumerate(self.dma_keys)}
            block = st.enter_context(nc.Block())
            total_waits = [0]

            def run(e, eng):
                seen = {}
                for i, rec in enumerate(self.ins[e]):
                    need = {}
                    for d in rec["deps"]:
                        if d[0] == "c":
                            s, v = esem[d[1]], semval[(d[1], d[2])]
                            k = ("c", d[1])
                        else:
                            s, v = dsem[d[1]], 16 * d[2]
                            k = ("d", d[1])
                        if seen.get(k, 0) >= v:
                            continue
                        if k not in need or need[k][1] < v:
                            need[k] = (s, v)
                    for k, (s, v) in need.items():
                        eng.wait_ge(s, v)
                        seen[k] = v
                        total_waits[0] += 1
                    ins = rec["fn"](eng)
                    if rec["dma"] is not None:
                        ins.then_inc(dsem[rec["dma"]], 16)
                    elif rec["flag"]:
                        ins.then_inc(esem[e], 1)
                if e == "sp":
                    for k in self.dma_keys:
                        eng.wait_ge(dsem[k], 16 * self.dma_count[k])

            @block.tensor
            def _(eng):
                run("pe", eng)

            @block.scalar
            def _(eng):
                run("act", eng)

            @block.vector
            def _(eng):
                run("dve", eng)

            @block.gpsimd
            def _(eng):
                run("pool", eng)

            @block.sync
            def _(eng):
                run("sp", eng)
        self.n_instr = {e: len(self.ins[e]) for e in ENGS}
        self.n_waits = total_waits[0]


A_Q, A_K, A_V, I_Q, I_K, I_W, C_Q, C_KV, K_R, GATE = 0, 512, 576, 640, 896, 960, 964, 1220, 1348, 1380
NB1 = 29


def w1_block_cols():
    blocks = []
    for c in range(4):
        blocks.append(np.arange(A_Q + 128 * c, A_Q + 128 * c + 128))
    blocks.append(np.concatenate([np.arange(A_K, A_K + 64)] * 2))
    for c in range(2):
        blocks.append(np.arange(I_Q + 128 * c, I_Q + 128 * c + 128))
    blocks.append(np.concatenate([np.arange(I_K, I_K + 64)] * 2))
    for c in range(2):
        blocks.append(np.arange(C_Q + 128 * c, C_Q + 128 * c + 128))
    blocks.append(np.arange(C_KV, C_KV + 128))
    kr = np.arange(K_R, K_R + 32)
    blocks.append(np.concatenate([kr] * 4))
    krs = np.concatenate([kr[16:], kr[:16]])
    blocks.append(np.concatenate([krs] * 4))
    for c in range(16):
        blocks.append(np.arange(GATE + 128 * c, GATE + 128 * c + 128))
    assert len(blocks) == NB1
    return blocks


def rel_bucket_np(n):
    n = np.maximum(n, 0)
    nf = np.maximum(n.astype(np.float32), np.float32(1.0))
    lb = 16 + (np.log(nf / np.float32(16)) / np.float32(math.log(8.0)) * np.float32(16)).astype(np.int32)
    return np.where(n < 16, n, np.minimum(lb, 31))


class Builder:
    def __init__(self, L, NL, NSEQ):
        self.L, self.NL, self.NSEQ = L, NL, NSEQ
        self.NT = L // 128
        self.NCH = L // 512
        self.KTOP = min(256, L // 4)
        self.gbc = 0
        self.gb_n = 4
        self.ibc = 0
        self.slotc = 0
        self.tmpc = 0
        self.ptc = 0
        self.ptmc = 0
        self.gbufc = 0
        self.pstc = 0

    def mm(self, out, lhsT, rhs, start, stop, R, W):
        self.P.op("pe", lambda e: e.matmul(out, lhsT=lhsT, rhs=rhs, start=start, stop=stop), R, W)

    def tr(self, out, in_, ident, R, W):
        self.P.op("pe", lambda e: e.transpose(out, in_, ident), R, W)

    def act(self, out, in_, func, R, W, bias=None, scale=None):
        kw = {}
        if bias is not None:
            kw["bias"] = bias
        if scale is not None:
            kw["scale"] = scale
        self.P.op("act", lambda e: e.activation(out=out, in_=in_, func=func, **kw), R, W)

    def ts(self, eng, out, in0, s1, s2, op0, op1, R, W, accum=None):
        kw = {}
        if op1 is not None:
            kw["op1"] = op1
        if accum is not None:
            kw["accum_out"] = accum
        self.P.op(eng, lambda e: e.tensor_scalar(out=out, in0=in0, scalar1=s1, scalar2=s2, op0=op0, **kw), R, W)

    def tt(self, eng, out, in0, in1, op, R, W):
        self.P.op(eng, lambda e: e.tensor_tensor(out=out, in0=in0, in1=in1, op=op), R, W)

    def stt(self, out, in0, scalar, in1, op0, op1, R, W):
        self.P.op("dve", lambda e: e.scalar_tensor_tensor(out=out, in0=in0, scalar=scalar, in1=in1, op0=op0, op1=op1), R, W)

    def cp(self, eng, out, in_, R, W):
        if eng == "act":
            self.P.op("act", lambda e: e.activation(out=out, in_=in_, func=AF.Copy), R, W)
        else:
            self.P.op(eng, lambda e: e.tensor_copy(out=out, in_=in_), R, W)

    def memset(self, eng, ap, val, W):
        self.P.op(eng, lambda e: e.memset(ap, val), (), W)

    def dma(self, q, out, in_, key, R, W):
        self.P.dma(q, lambda e: e.dma_start(out=out, in_=in_), key, R, W)

    def gb(self):
        k = self.gbc % self.gb_n
        self.gbc += 1
        return self.ps[k], "ps%d" % k

    def ib(self):
        k = (3, 6)[self.ibc % 2]
        self.ibc += 1
        return self.ps[k], "ps%d" % k

    def tmpf(self):
        k = self.tmpc % len(self.tmp_aps)
        self.tmpc += 1
        return self.tmp_aps[k]

    def load_w(self, src, ncols):
        k = self.slotc % NSLOT
        self.slotc += 1
        self.dma("pool", self.wr[k][:, 0:ncols], src, "w%d" % k, (), ["w%d" % k])
        return self.wr[k], "w%d" % k

    def HR(self, c, nt=None):
        if nt is None:
            return ["H%d_%d" % (c, n) for n in range(self.NCH)]
        return ["H%d_%d" % (c, nt)]

    def UR(self, c, nt=None):
        if nt is None:
            return ["U%d_%d" % (c, n) for n in range(self.NCH)]
        return ["U%d_%d" % (c, nt)]

    def XR(self, c, nt):
        return ["X%d_%d" % (c, nt)]

    def Hv(self, c, a, b):
        return self.H[:, c * self.L + a: c * self.L + b]

    def Uv(self, c, a, b):
        return self.U[:, c * self.L + a: c * self.L + b]

    def rmsnorm_nt(self, srcs, src_res, g_aps, dim, dsts, dst_res):
        n = len(srcs)
        for c in range(n):
            sq, sqr = self.tmpf()
            self.act(sq, srcs[c], AF.Square, src_res[c], [sqr])
            self.mm(self.ps[6][:, :], self.ones_f[:, :], sq, c == 0, c == n - 1, [sqr, "ones"], ["ps6"])
        self.act(self.rstd[:, :], self.ps[6][:, :], AF.Sqrt, ["ps6", "epsb"], ["rstd"], bias=self.epsb[:, 0:1], scale=1.0 / dim)
        self.P.op("dve", lambda e: e.reciprocal(out=self.rstd[:, :], in_=self.rstd[:, :]), ["rstd"], ["rstd"])
        for c in range(n):
            self.stt(dsts[c], srcs[c], g_aps[c], self.rstd[:, :], ALU.mult, ALU.mult, list(src_res[c]) + ["rstd", "gains"], dst_res[c])

    def build(self):
        L, NL, NSEQ, NT, NCH = self.L, self.NL, self.NSEQ, self.NT, self.NCH
        nc = bass.Bass("TRN2", target_bir_lowering=False)
        self.nc = nc

        def din(name, shape, dt=F32):
            return nc.dram_tensor(name, shape, dt, kind="ExternalInput").ap()

        x = din("x", [NSEQ, L, D])
        pos = din("pos", [NSEQ, 1, L], I32)
        gains = din("gains", [128, NL * 35 + 8])
        w1b = din("w1b", [NL, NB1, 128, 1024])
        w1t = din("w1t", [NL, 128, 8 * 68])
        wuq = din("wuq", [NL, 128, 2 * 768])
        wuqs = din("wuqs", [NL, 128, 2 * 768])
        wukv = din("wukv", [NL, 128, 1024])
        wa = din("wa", [NL, 8, 128, 512])
        wb = din("wb", [NL, 8, 128, 512])
        wo = din("wo", [NL, 8, 128, 1024])
        wf1 = din("wf1", [NL, 32, 128, 1024])
        wf2 = din("wf2", [NL, 32, 128, 1024])
        band_d = din("band", [8, 128, 256])
        rb31_d = din("rb31", [1, 8])
        cmats = din("cmats", [5, 128, 128])
        rconst = din("rconst", [128, 4])
        y = nc.dram_tensor("y", [NSEQ, L, D], F32, kind="ExternalOutput").ap()
        gsc = nc.dram_tensor("gsc", [16, 128, L], BF16, kind="Internal").ap()
        lat = nc.dram_tensor("lat", [5, 128, L], BF16, kind="Internal").ap()

        with ExitStack() as st:
            def sb(name, shape, dt):
                return st.enter_context(nc.sbuf_tensor(name, shape, dt))

            def psum(name, shape, dt):
                return st.enter_context(nc.psum_tensor(name, shape, dt))

            XT = sb("XT", [128, 8, L], F32)
            self.H = sb("H", [128, 8 * L], BF16)
            self.U = sb("U", [128, 8 * L], BF16)
            self.wr = [sb("wr%d" % k, [128, 1024], BF16) for k in range(NSLOT)]
            wuq_sb = sb("wuq_sb", [128, 1536], BF16)
            wuqs_sb = sb("wuqs_sb", [128, 1536], BF16)
            wukv_sb = sb("wukv_sb", [128, 1024], BF16)
            w1t_sb = sb("w1t_sb", [128, 8 * 68], BF16)
            va = sb("va", [128, NT, 65], BF16)
            vb = [sb("vb%d" % k, [128, NT, 65], BF16) for k in range(2)]
            iwabs = sb("iwabs", [128, NT, 4], F32)
            iwsgn = sb("iwsgn", [128, NT, 4], F32)
            score = sb("score", [128, max(L, 1024)], F32)
            Mts = sb("Mts", [128, L], BF16)
            MT = [sb("MT%d" % k, [128, L], BF16) for k in range(2)]
            PT = [sb("PT%d" % k, [128, 512], BF16) for k in range(2)]
            PTm = [sb("PTm%d" % k, [128, 512], BF16) for k in range(2)]
            self.tmp = [sb("tmp%d" % k, [128, 512], F32) for k in range(2)]
            self.rstd = sb("rstd", [128, 512], F32)
            ytok = [sb("ytok%d" % k, [128, 512], BF16) for k in range(2)]
            cosT = sb("cosT", [128, L], BF16)
            sinT = sb("sinT", [128, L], BF16)
            gbuf = [sb("gbuf%d" % k, [128, 512], BF16) for k in range(4)]
            band = sb("band_sb", [128, 8, 256], BF16)
            ident_f = sb("ident_f", [128, 128], F32)
            ident_b = sb("ident_b", [128, 128], BF16)
            negtri_b = sb("negtri_b", [128, 128], BF16)
            negtri2 = sb("negtri2", [128, 128], F32)
            tril_b = sb("tril_b", [128, 128], BF16)
            self.ones_f = sb("ones_f", [128, 128], F32)
            self.epsb = sb("epsb", [128, 1], F32)
            gains_sb = sb("gains_sb", [128, NL * 35 + 8], F32)
            rb31 = sb("rb31_sb", [128, 8], F32)
            rb31n = sb("rb31n", [128, 8], F32)
            rconst_sb = sb("rconst_sb", [128, 4], F32)
            thr = sb("thr", [128, 1], F32)
            cnt = sb("cnt", [128, 1], F32)
            uu = sb("uu", [128, 1], F32)
            rec = sb("rec", [128, 8], F32)
            posi = sb("posi", [128, 512], I32)
            ki = sb("ki", [128, 512], I32)
            self.ps = [psum("ps%d" % k, [128, 512], F32) for k in range(7)]
            self.tmp_aps = [(self.tmp[0][:, :], "tmp0"), (self.tmp[1][:, :], "tmp1"),
                            (posi[:, :].bitcast(F32), "posi"), (ki[:, :].bitcast(F32), "ki")]
            pst = psum("pst", [128, 1024], BF16)

            P = Prog(nc)
            self.P = P
            ps = self.ps

            def g_attn(l, c):
                return gains_sb[:, l * 35 + c: l * 35 + c + 1]

            def g_mlp(l, c):
                return gains_sb[:, l * 35 + 8 + c: l * 35 + 9 + c]

            def g_q(l, c):
                return gains_sb[:, l * 35 + 16 + c: l * 35 + 17 + c]

            def g_kv(l):
                return gains_sb[:, l * 35 + 18: l * 35 + 19]

            def b_gate(l, c):
                return gains_sb[:, l * 35 + 19 + c: l * 35 + 20 + c]

            def g_fin(c):
                return gains_sb[:, NL * 35 + c: NL * 35 + c + 1]

            self.dma("sp", gains_sb[:, :], gains[:, :], "c_gains", (), ["gains"])
            self.dma("sp", ident_f[:, :], cmats[0, :, :], "c_identf", (), ["identf"])
            self.dma("pool", ident_b[:, :], cmats[0, :, :], "c_identb", (), ["identb"])
            self.dma("pool", negtri_b[:, :], cmats[1, :, :], "c_negtri", (), ["negtri"])
            self.dma("sp", negtri2[:, :], cmats[2, :, :], "c_negtri2", (), ["negtri2"])
            self.dma("pool", tril_b[:, :], cmats[3, :, :], "c_tril", (), ["tril"])
            self.dma("sp", rconst_sb[:, :], rconst[:, :], "c_rconst", (), ["rconst"])
            self.dma("sp", rb31[:, :], rb31_d.partition_broadcast(128), "c_rb31", (), ["rb31"])
            self.memset("dve", self.ones_f[:, :], 1.0, ["ones"])
            self.memset("dve", self.epsb[:, :], EPS, ["epsb"])
            self.memset("dve", va[:, :, 64:65], 1.0, ["va"])
            self.memset("dve", vb[0][:, :, 64:65], 1.0, ["vb0"])
            self.memset("dve", vb[1][:, :, 64:65], 1.0, ["vb1"])
            self.ts("dve", rb31n[:, :], rb31[:, :], -1.0, None, ALU.mult, None, ["rb31"], ["rb31n"])
            for h in range(8):
                self.dma("sp", self.rstd[:, 0:256], band_d[h, :, :], "c_bandf", (), ["rstd"])
                self.ts("dve", band[:, h, :], self.rstd[:, 0:256], rb31n[:, h:h + 1], None, ALU.add, None, ["rstd", "rb31n"], ["band"])

            for s in range(NSEQ):
                self.seq(s, x, pos, y, XT, gsc, lat, locals())
            P.emit()
            self.stats = (P.n_instr, P.n_waits)
        return nc

    def seq(self, s, x, pos, y, XT, gsc, lat, env):
        L, NL, NT, NCH = self.L, self.NL, self.NT, self.NCH
        ps = self.ps
        P = self.P
        g = env
        score, Mts, MT, PT, PTm = g["score"], g["Mts"], g["MT"], g["PT"], g["PTm"]
        ident_f, ident_b, negtri_b, negtri2, tril_b = g["ident_f"], g["ident_b"], g["negtri_b"], g["negtri2"], g["tril_b"]
        cosT, sinT, rconst_sb, posi, ki = g["cosT"], g["sinT"], g["rconst_sb"], g["posi"], g["ki"]
        pst, va, vb, iwabs, iwsgn = g["pst"], g["va"], g["vb"], g["iwabs"], g["iwsgn"]
        thr, cnt, uu, rec, rb31, band = g["thr"], g["cnt"], g["uu"], g["rec"], g["rb31"], g["band"]
        ytok, gbuf = g["ytok"], g["gbuf"]
        wuq_sb, wuqs_sb, wukv_sb, w1t_sb = g["wuq_sb"], g["wuqs_sb"], g["wukv_sb"], g["w1t_sb"]
        w1b, w1t, wuq, wuqs, wukv, wa, wb, wo, wf1, wf2 = (g[k] for k in ("w1b", "w1t", "wuq", "wuqs", "wukv", "wa", "wb", "wo", "wf1", "wf2"))
        g_attn, g_mlp, g_q, g_kv, b_gate, g_fin = g["g_attn"], g["g_mlp"], g["g_q"], g["g_kv"], g["b_gate"], g["g_fin"]
        KTOP = self.KTOP

        for nt in range(NCH):
            cs = slice(nt * 512, nt * 512 + 512)
            self.dma("sp", posi[:, :], pos[s, :, cs].partition_broadcast(128), "posi", (), ["posi"])
            for tab, c0, name in ((cosT, 0, "cosT"), (sinT, 2, "sinT")):
                t0, r0 = self.tmp[0][:, :], "tmp0"
                self.cp("dve", t0, posi[:, :], ["posi"], [r0])
                t1, r1 = self.tmp[1][:, :], "tmp1"
                self.ts("dve", t1, t0, rconst_sb[:, c0:c0 + 1], rconst_sb[:, c0 + 1:c0 + 2], ALU.mult, ALU.add, [r0, "rconst"], [r1])
                self.cp("dve", ki[:, :], t1, [r1], ["ki"])
                self.cp("dve", t0, ki[:, :], ["ki"], [r0])
                self.tt("dve", t1, t1, t0, ALU.subtract, [r0, r1], [r1])
                self.act(tab[:, cs], t1, AF.Sin, [r1], [name], scale=6.2831845)

        for i in range(NT):
            sg = score[:, 0:1024]
            self.dma("sp", sg, x[s, i * 128:(i + 1) * 128, :], "xin", (), ["score"])
            for half in range(2):
                bank, br = self.gb()
                for c4 in range(4):
                    c = half * 4 + c4
                    self.tr(bank[:, c4 * 128:(c4 + 1) * 128], sg[:, c * 128:(c + 1) * 128], ident_f[:, :], ["score", "identf"], [br])
                dst = XT[:, half * 4:half * 4 + 4, i * 128:(i + 1) * 128]
                src = bank[:, :].rearrange("p (c t) -> p c t", c=4)
                self.cp("act", dst, src, [br], ["X%d_%d" % (c, i // 4) for c in range(half * 4, half * 4 + 4)])

        for l in range(NL):
            self.layer(s, l, env)

        for nt in range(NCH):
            cs = slice(nt * 512, nt * 512 + 512)
            for c in range(8):
                sq, sqr = self.tmpf()
                self.act(sq, XT[:, c, cs], AF.Square, self.XR(c, nt), [sqr])
                self.mm(ps[6][:, :], self.ones_f[:, :], sq, c == 0, c == 7, [sqr, "ones"], ["ps6"])
            self.act(self.rstd[:, :], ps[6][:, :], AF.Sqrt, ["ps6", "epsb"], ["rstd"], bias=self.epsb[:, 0:1], scale=1.0 / D)
            P.op("dve", lambda e: e.reciprocal(out=self.rstd[:, :], in_=self.rstd[:, :]), ["rstd"], ["rstd"])
            for ti in range(4):
                i = nt * 4 + ti
                sg = score[:, 0:1024]
                for half in range(2):
                    bank, br = self.gb()
                    for c4 in range(4):
                        c = half * 4 + c4
                        t0, r0 = self.tmpf()
                        self.stt(t0[:, 0:128], XT[:, c, i * 128:(i + 1) * 128], g_fin(c), self.rstd[:, ti * 128:(ti + 1) * 128],
                                 ALU.mult, ALU.mult, self.XR(c, nt) + ["rstd", "gains"], [r0])
                        self.tr(bank[:, c4 * 128:(c4 + 1) * 128], t0[:, 0:128], ident_f[:, :], [r0, "identf"], [br])
                    self.cp("act", sg[:, half * 512:(half + 1) * 512], bank[:, :], [br], ["score"])
                self.dma("sp", y[s, i * 128:(i + 1) * 128, :], sg, "yout", ["score"], ())

    def layer(self, s, l, env):
        L, NL, NT, NCH = self.L, self.NL, self.NT, self.NCH
        ps = self.ps
        P = self.P
        g = env
        XT = g["XT"]
        score, Mts, MT, PT, PTm = g["score"], g["Mts"], g["MT"], g["PT"], g["PTm"]
        ident_f, ident_b, negtri_b, negtri2, tril_b = g["ident_f"], g["ident_b"], g["negtri_b"], g["negtri2"], g["tril_b"]
        cosT, sinT = g["cosT"], g["sinT"]
        pst, va, vb, iwabs, iwsgn = g["pst"], g["va"], g["vb"], g["iwabs"], g["iwsgn"]
        thr, cnt, uu, rec, rb31, band = g["thr"], g["cnt"], g["uu"], g["rec"], g["rb31"], g["band"]
        ytok, gbuf = g["ytok"], g["gbuf"]
        gsc, lat = g["gsc"], g["lat"]
        wuq_sb, wuqs_sb, wukv_sb, w1t_sb = g["wuq_sb"], g["wuqs_sb"], g["wukv_sb"], g["w1t_sb"]
        w1b, w1t, wuq, wuqs, wukv, wa, wb, wo, wf1, wf2 = (g[k] for k in ("w1b", "w1t", "wuq", "wuqs", "wukv", "wa", "wb", "wo", "wf1", "wf2"))
        g_attn, g_mlp, g_q, g_kv, b_gate = g["g_attn"], g["g_mlp"], g["g_q"], g["g_kv"], g["b_gate"]
        KTOP = self.KTOP
        Hv, Uv, HR, UR, XR = self.Hv, self.Uv, self.HR, self.UR, self.XR

        self.dma("pool", wuq_sb[:, :], wuq[l, :, :], "wuq", (), ["wuq"])
        self.dma("pool", wuqs_sb[:, :], wuqs[l, :, :], "wuqs", (), ["wuqs"])
        self.dma("pool", wukv_sb[:, :], wukv[l, :, :], "wukv", (), ["wukv"])
        self.dma("pool", w1t_sb[:, :], w1t[l, :, :], "w1t", (), ["w1t"])

        for nt in range(NCH):
            cs = (nt * 512, nt * 512 + 512)
            self.rmsnorm_nt([XT[:, c, cs[0]:cs[1]] for c in range(8)], [XR(c, nt) for c in range(8)],
                            [g_attn(l, c) for c in range(8)], D,
                            [Hv(c, *cs) for c in range(8)], [HR(c, nt) for c in range(8)])

        def evac_plain(unit):
            def f(bank, br, nt):
                self.cp("act", Uv(unit, nt * 512, nt * 512 + 512), bank[:, :], [br], UR(unit, nt))
            return f

        def evac_q(unit):
            def f(bank, br, nt):
                self.act(Uv(unit, nt * 512, nt * 512 + 512), bank[:, :], AF.Copy, [br], UR(unit, nt), scale=0.125)
            return f

        def evac_lat(k):
            def f(bank, br, nt):
                gbt = gbuf[self.gbufc % 4]
                gr = "gbuf%d" % (self.gbufc % 4)
                self.gbufc += 1
                self.cp("act", gbt[:, :], bank[:, :], [br], [gr])
                self.dma("sp", lat[k, :, nt * 512:nt * 512 + 512], gbt[:, :], "st_" + gr, [gr], ["lat%d" % k])
            return f

        def evac_gate(c):
            def f(bank, br, nt):
                gbt = gbuf[self.gbufc % 4]
                gr = "gbuf%d" % (self.gbufc % 4)
                self.gbufc += 1
                self.act(gbt[:, :], bank[:, :], AF.Sigmoid, [br, "gains"], [gr], bias=b_gate(l, c))
                self.dma("sp", gsc[c, :, nt * 512:nt * 512 + 512], gbt[:, :], "st_" + gr, [gr], ["gsc%d" % c])
            return f

        evacs = [evac_q(0), evac_q(1), evac_q(2), evac_q(3), evac_plain(6), evac_plain(4), evac_plain(5), evac_plain(7),
                 evac_lat(0), evac_lat(1), evac_lat(2), evac_lat(3), evac_lat(4)] + [evac_gate(c) for c in range(16)]
        for b in range(NB1):
            wt, wres = self.load_w(w1b[l, b, :, :], 1024)
            for nt in range(NCH):
                bank, br = self.gb()
                for kc in range(8):
                    self.mm(bank[:, :], wt[:, kc * 128:(kc + 1) * 128], Hv(kc, nt * 512, nt * 512 + 512), kc == 0, kc == 7,
                            [wres] + HR(kc, nt), [br])
                evacs[b](bank, br, nt)
        for i in range(NT):
            bank, br = self.gb()
            for kc in range(8):
                self.mm(bank[:, 0:68], Hv(kc, i * 128, (i + 1) * 128), w1t_sb[:, kc * 68:(kc + 1) * 68], kc == 0, kc == 7,
                        ["w1t"] + HR(kc, i // 4), [br])
            self.cp("act", va[:, i, 0:64], bank[:, 0:64], [br], ["va"])
            self.act(iwabs[:, i, :], bank[:, 64:68], AF.Abs, [br], ["iw"], scale=0.0625)
            self.act(iwsgn[:, i, :], bank[:, 64:68], AF.Sign, [br], ["iw"])

        def indexer(i):
            S = 128 * (i + 1)
            for kb in range((S + 511) // 512):
                c0, c1 = kb * 512, min(S, kb * 512 + 512)
                w = c1 - c0
                for h in range(4):
                    unit = 4 + h // 2
                    r0, r1 = (h % 2) * 64, (h % 2) * 64 + 64
                    bank, br = self.ib()
                    self.mm(bank[:, 0:w], self.U[r0:r1, unit * L + i * 128: unit * L + (i + 1) * 128],
                            self.U[r0:r1, 7 * L + c0: 7 * L + c1], True, True,
                            UR(unit, i // 4) + [r for k in range(c0 // 512, (c1 + 511) // 512) for r in UR(7, k)], [br])
                    t0, tr0 = self.tmpf()
                    self.act(t0[:, 0:w], bank[:, 0:w], AF.Relu, [br, "iw"], [tr0], scale=iwabs[:, i, h:h + 1])
                    if h == 0:
                        self.ts("dve", score[:, c0:c1], t0[:, 0:w], iwsgn[:, i, h:h + 1], None, ALU.mult, None, [tr0, "iw"], ["score"])
                    else:
                        self.stt(score[:, c0:c1], t0[:, 0:w], iwsgn[:, i, h:h + 1], score[:, c0:c1], ALU.mult, ALU.add,
                                 [tr0, "iw", "score"], ["score"])
            self.tt("dve", score[:, i * 128:(i + 1) * 128], score[:, i * 128:(i + 1) * 128], negtri2[:, :], ALU.add,
                    ["score", "negtri2"], ["score"])

        def bisect(i):
            S = 128 * (i + 1)
            if S <= KTOP:
                if i > 0:
                    self.memset("dve", Mts[:, 0:i * 128], 1.0, ["Mts"])
                self.cp("dve", Mts[:, i * 128:(i + 1) * 128], tril_b[:, :], ["tril"], ["Mts"])
                return
            self.memset("dve", thr[:, :], 0.0, ["thr"])
            for n in range(NIT):
                stp = LIM / (2.0 ** (n + 1))
                self.ts("dve", Mts[:, 0:S], score[:, 0:S], thr[:, 0:1], 0.0, ALU.is_gt, ALU.add, ["score", "thr"], ["Mts", "cnt"], accum=cnt[:, 0:1])
                self.ts("dve", uu[:, :], cnt[:, :], KTOP - 0.5, 2.0 * stp, ALU.is_ge, ALU.mult, ["cnt"], ["uu"])
                self.stt(thr[:, :], thr[:, :], -stp, uu[:, :], ALU.add, ALU.add, ["thr", "uu"], ["thr"])
            self.ts("dve", Mts[:, 0:S], score[:, 0:S], thr[:, 0:1], None, ALU.is_gt, None, ["score", "thr"], ["Mts"])

        def masktrans(i, buf):
            for g0 in range(0, i + 1, 4):
                jn = min(4, i + 1 - g0)
                half = self.pstc % 2
                self.pstc += 1
                pr = "pst"
                for jj in range(jn):
                    j = g0 + jj
                    self.tr(pst[:, half * 512 + jj * 128: half * 512 + (jj + 1) * 128], Mts[:, j * 128:(j + 1) * 128], ident_b[:, :],
                            ["Mts", "identb"], [pr])
                self.cp("act", MT[buf][:, g0 * 128:(g0 + jn) * 128], pst[:, half * 512: half * 512 + jn * 128], [pr], ["MT%d" % buf])

        def attn_items(i, mixer, head, kT, kres, qT, qres, kdim, vt, vres, acc, accres, acol, mtbuf):
            items = []
            for g0 in range(0, i + 1, 4):
                jn = min(4, i + 1 - g0)
                w = jn * 128
                st = {}

                def qk(g0=g0, jn=jn, st=st):
                    bank, br = self.gb()
                    st["bank"], st["br"] = bank, br
                    for jj in range(jn):
                        j = g0 + jj
                        near = (j >= i - 1) if mixer == "A" else (j == i)
                        self.mm(bank[:, jj * 128:(jj + 1) * 128], kT(j), qT(i), True, not near, kres(j) + qres(i), [br])
                        if near:
                            if mixer == "A":
                                rhs = band[:, head, (i - j) * 128:(i - j) * 128 + 128]
                                rr = ["band"]
                            else:
                                rhs = negtri_b[:, :]
                                rr = ["negtri"]
                            self.mm(bank[:, jj * 128:(jj + 1) * 128], ident_b[:, :], rhs, False, True, ["identb"] + rr, [br])

                def mid(g0=g0, w=w, st=st):
                    bank, br = st["bank"], st["br"]
                    pk = self.ptc % 2
                    self.ptc += 1
                    if mixer == "A":
                        self.act(PT[pk][:, 0:w], bank[:, 0:w], AF.Exp, [br, "rb31"], ["PT%d" % pk], bias=rb31[:, head:head + 1])
                        mk = self.ptmc % 2
                        self.ptmc += 1
                        self.tt("pool", PTm[mk][:, 0:w], PT[pk][:, 0:w], MT[mtbuf][:, g0 * 128: g0 * 128 + w], ALU.mult,
                                ["PT%d" % pk, "MT%d" % mtbuf], ["PTm%d" % mk])
                        st["pt"], st["ptr"] = PTm[mk], "PTm%d" % mk
                    else:
                        self.act(PT[pk][:, 0:w], bank[:, 0:w], AF.Exp, [br], ["PT%d" % pk], scale=96.0 ** -0.5)
                        st["pt"], st["ptr"] = PT[pk], "PT%d" % pk

                def pv(g0=g0, jn=jn, st=st):
                    pt, ptr = st["pt"], st["ptr"]
                    for jj in range(jn):
                        j = g0 + jj
                        self.mm(acc[:, acol:acol + 65], pt[:, jj * 128:(jj + 1) * 128], vt[:, j, :], j == 0, j == i, [ptr, vres], [accres])

                items.append(dict(qk=qk, mid=mid, pv=pv, post=None))
            return items

        def run_items(items, PD=3, hook=None):
            n = len(items)
            PD = min(PD, self.gb_n - 1)
            hk = (3 * n) // 4
            for j in range(min(PD, n)):
                items[j]["qk"]()
            for k, it in enumerate(items):
                if k + PD < n:
                    items[k + PD]["qk"]()
                it["mid"]()
                it["pv"]()
                if it["post"] is not None:
                    it["post"]()
                if hook is not None and k == hk:
                    hook()
                    hook = None
            if hook is not None:
                hook()

        def y_transposes(src_tile, src_res, dst_fn, dst_res):
            half = self.pstc % 2
            self.pstc += 1
            pr = "pst"
            for c in range(4):
                self.tr(pst[:, half * 512 + c * 128: half * 512 + (c + 1) * 128], src_tile[:, c * 128:(c + 1) * 128], ident_b[:, :],
                        [src_res, "identb"], [pr])
            self.cp("act", dst_fn, pst[:, half * 512: half * 512 + 512].rearrange("p (c t) -> p c t", c=4), [pr], dst_res)

        Hy = self.H[:, :].rearrange("p (c t) -> p c t", c=8)
        Uy = self.U[:, :].rearrange("p (c t) -> p c t", c=8)

        def attnA(i, buf, hook=None):
            items = []
            for h in range(8):
                c = h // 2
                r0, r1 = (h % 2) * 64, (h % 2) * 64 + 64
                acc, accres = (ps[4], "ps4") if h < 4 else (ps[5], "ps5")
                items += attn_items(i, "A", h,
                                    lambda j, r0=r0, r1=r1: self.U[r0:r1, 6 * L + j * 128: 6 * L + (j + 1) * 128], lambda j: UR(6, j // 4),
                                    lambda ii, r0=r0, r1=r1, c=c: self.U[r0:r1, c * L + ii * 128: c * L + (ii + 1) * 128],
                                    lambda ii, c=c: UR(c, ii // 4),
                                    64, va, "va", acc, accres, (h % 4) * 65, buf)
            run_items(items, hook=hook)
            yt = ytok[i % 2]
            yr = "ytok%d" % (i % 2)
            for hh, (acc, accres) in enumerate(((ps[4], "ps4"), (ps[5], "ps5"))):
                a3 = acc[:, 0:260].rearrange("p (h d) -> p h d", d=65)
                self.act(rec[:, hh * 4:hh * 4 + 4], a3[:, :, 64], AF.Ln, [accres], ["rec"])
                self.act(rec[:, hh * 4:hh * 4 + 4], rec[:, hh * 4:hh * 4 + 4], AF.Exp, ["rec"], ["rec"], scale=-1.0)
                for h4 in range(4):
                    h = hh * 4 + h4
                    self.act(yt[:, h * 64:(h + 1) * 64], acc[:, h4 * 65:h4 * 65 + 64], AF.Copy, [accres, "rec"], [yr], scale=rec[:, h:h + 1])
            y_transposes(yt, yr, Hy[:, 0:4, i * 128:(i + 1) * 128], [r for c in range(4) for r in HR(c, i // 4)])

        self.gb_n = 3
        indexer(0)
        bisect(0)
        masktrans(0, 0)
        if NT > 1:
            indexer(1)
            bisect(1)

        def mk_hook(i):
            def hook():
                if i + 1 < NT:
                    masktrans(i + 1, (i + 1) % 2)
                if i + 2 < NT:
                    indexer(i + 2)
                    bisect(i + 2)
            return hook

        for i in range(NT):
            attnA(i, i % 2, mk_hook(i))
        self.gb_n = 4

        for k, unit in ((0, 0), (1, 1), (2, 2), (3, 3), (4, 4)):
            self.dma("sp", Uv(unit, 0, L), lat[k, :, :], "latld%d" % k, ["lat%d" % k], UR(unit))
        for nt in range(NCH):
            cs = (nt * 512, nt * 512 + 512)
            self.rmsnorm_nt([Uv(0, *cs), Uv(1, *cs)], [UR(0, nt), UR(1, nt)], [g_q(l, 0), g_q(l, 1)], 256,
                            [Uv(5, *cs), Uv(6, *cs)], [UR(5, nt), UR(6, nt)])
            self.rmsnorm_nt([Uv(2, *cs)], [UR(2, nt)], [g_kv(l)], 128, [Uv(7, *cs)], [UR(7, nt)])
            t0, r0 = self.tmpf()
            t1, r1 = self.tmpf()
            R6 = slice(64, 96)
            self.tt("dve", t0[R6, :], self.U[R6, 3 * L + cs[0]: 3 * L + cs[1]], cosT[R6, cs[0]:cs[1]], ALU.mult, UR(3, nt) + ["cosT"], [r0])
            self.tt("dve", t1[R6, :], self.U[R6, 4 * L + cs[0]: 4 * L + cs[1]], sinT[R6, cs[0]:cs[1]], ALU.mult, UR(4, nt) + ["sinT"], [r1])
            self.tt("dve", self.U[R6, 3 * L + cs[0]: 3 * L + cs[1]], t0[R6, :], t1[R6, :], ALU.add, [r0, r1], UR(3, nt))

        Ytb = self.H[:, 4 * L: 8 * L].rearrange("p (i f) -> p i f", f=512)
        def mla_proj(h):
            qb = h % 2
            kb_ = 2 if h % 2 == 0 else 4
            vbuf = vb[h % 2]
            vres = "vb%d" % (h % 2)
            R6 = slice(64, 96)
            for nt in range(NCH):
                cs = (nt * 512, nt * 512 + 512)
                bA, brA = self.gb()
                bB, brB = self.gb()
                for kc in range(2):
                    self.mm(bA[0:96, :], wuq_sb[:, kc * 768 + 96 * h: kc * 768 + 96 * h + 96], Uv(5 + kc, *cs), kc == 0, kc == 1,
                            ["wuq"] + UR(5 + kc, nt), [brA])
                for kc in range(2):
                    self.mm(bB[0:96, :], wuqs_sb[:, kc * 768 + 96 * h: kc * 768 + 96 * h + 96], Uv(5 + kc, *cs), kc == 0, kc == 1,
                            ["wuqs"] + UR(5 + kc, nt), [brB])
                self.cp("act", self.U[0:64, qb * L + cs[0]: qb * L + cs[1]], bA[0:64, :], [brA], UR(qb, nt))
                t0, r0 = self.tmpf()
                t1, r1 = self.tmpf()
                self.tt("dve", t0[R6, :], bA[R6, :], cosT[R6, cs[0]:cs[1]], ALU.mult, [brA, "cosT"], [r0])
                self.tt("dve", t1[R6, :], bB[R6, :], sinT[R6, cs[0]:cs[1]], ALU.mult, [brB, "sinT"], [r1])
                self.tt("dve", self.U[R6, qb * L + cs[0]: qb * L + cs[1]], t0[R6, :], t1[R6, :], ALU.add, [r0, r1], UR(qb, nt))
                bK, brK = self.gb()
                self.mm(bK[0:64, :], wukv_sb[:, 128 * h: 128 * h + 64], Uv(7, *cs), True, True, ["wukv"] + UR(7, nt), [brK])
                self.cp("act", self.U[0:64, kb_ * L + cs[0]: kb_ * L + cs[1]], bK[0:64, :], [brK], UR(kb_, nt))
                self.cp("pool", self.U[R6, kb_ * L + cs[0]: kb_ * L + cs[1]], self.U[R6, 3 * L + cs[0]: 3 * L + cs[1]], UR(3, nt), UR(kb_, nt))
            for i0 in range(0, NT, 4):
                bV, brV = self.gb()
                for ii in range(4):
                    i = i0 + ii
                    self.mm(bV[:, ii * 64:(ii + 1) * 64], Uv(7, i * 128, (i + 1) * 128), wukv_sb[:, 128 * h + 64: 128 * h + 128], True, True,
                            ["wukv"] + UR(7, i // 4), [brV])
                self.cp("act", vbuf[:, i0:i0 + 4, 0:64], bV[:, 0:256].rearrange("p (i d) -> p i d", d=64), [brV], [vres])

        def mla_attn(h):
            qb = h % 2
            kb_ = 2 if h % 2 == 0 else 4
            vbuf = vb[h % 2]
            vres = "vb%d" % (h % 2)
            R6 = slice(64, 96)
            items = []
            for i0 in range(0, NT, 4):
                acc, accres = (ps[4], "ps4") if (i0 // 4) % 2 == 0 else (ps[5], "ps5")
                for ii in range(4):
                    i = i0 + ii
                    items += attn_items(i, "B", h,
                                        lambda j: self.U[0:96, kb_ * L + j * 128: kb_ * L + (j + 1) * 128], lambda j: UR(kb_, j // 4),
                                        lambda q: self.U[0:96, qb * L + q * 128: qb * L + (q + 1) * 128], lambda q: UR(qb, q // 4),
                                        96, vbuf, vres, acc, accres, ii * 65, 0)

                def post(i0=i0, acc=acc, accres=accres):
                    a3 = acc[:, 0:260].rearrange("p (h d) -> p h d", d=65)
                    P.op("dve", lambda e, a3=a3: e.reciprocal(out=rec[:, 0:4], in_=a3[:, :, 64]), [accres], ["rec"])
                    for ii in range(4):
                        i = i0 + ii
                        self.ts("dve", Ytb[:, i, h * 64:(h + 1) * 64], acc[:, ii * 65: ii * 65 + 64], rec[:, ii:ii + 1], None, ALU.mult, None,
                                [accres, "rec"], HR(4 + i // (NT // 4), None))
                items[-1]["post"] = post
            run_items(items)

        mla_proj(0)
        for h in range(8):
            if h + 1 < 8:
                mla_proj(h + 1)
            mla_attn(h)
        for i in range(NT):
            half = self.pstc % 2
            self.pstc += 1
            pr = "pst"
            for c in range(4):
                self.tr(pst[:, half * 512 + c * 128: half * 512 + (c + 1) * 128], Ytb[:, i, c * 128:(c + 1) * 128], ident_b[:, :],
                        HR(4 + i // (NT // 4), None) + ["identb"], [pr])
            self.cp("act", Uy[:, 0:4, i * 128:(i + 1) * 128], pst[:, half * 512: half * 512 + 512].rearrange("p (c t) -> p c t", c=4), [pr],
                    [r for c in range(4) for r in UR(c, i // 4)])

        def merged_view(oc, a, b):
            return Uv(4 + oc, a, b) if oc < 4 else Hv(oc, a, b)

        def merged_res(oc, nt):
            return UR(4 + oc, nt) if oc < 4 else HR(oc, nt)

        for oc in range(8):
            wA, wAr = self.load_w(wa[l, oc, :, :], 512)
            wB, wBr = self.load_w(wb[l, oc, :, :], 512)
            for nt in range(NCH):
                cs = (nt * 512, nt * 512 + 512)
                bA, brA = self.gb()
                for kc in range(4):
                    self.mm(bA[:, :], wA[:, kc * 128:(kc + 1) * 128], Hv(kc, *cs), kc == 0, kc == 3, [wAr] + HR(kc, nt), [brA])
                bB, brB = self.gb()
                for kc in range(4):
                    self.mm(bB[:, :], wB[:, kc * 128:(kc + 1) * 128], Uv(kc, *cs), kc == 0, kc == 3, [wBr] + UR(kc, nt), [brB])
                g0 = gbuf[self.gbufc % 4]
                g0r = "gbuf%d" % (self.gbufc % 4)
                self.gbufc += 1
                g1 = gbuf[self.gbufc % 4]
                g1r = "gbuf%d" % (self.gbufc % 4)
                self.gbufc += 1
                self.dma("sp", g0[:, :], gsc[oc, :, cs[0]:cs[1]], "ld_" + g0r, ["gsc%d" % oc], [g0r])
                self.dma("sp", g1[:, :], gsc[8 + oc, :, cs[0]:cs[1]], "ld_" + g1r, ["gsc%d" % (8 + oc)], [g1r])
                t0, r0 = self.tmpf()
                t1, r1 = self.tmpf()
                self.tt("dve", t0, bA[:, :], g0[:, :], ALU.mult, [brA, g0r], [r0])
                self.tt("dve", t1, bB[:, :], g1[:, :], ALU.mult, [brB, g1r], [r1])
                self.tt("dve", merged_view(oc, *cs), t0, t1, ALU.add, [r0, r1], merged_res(oc, nt))
        for oc in range(8):
            wO, wOr = self.load_w(wo[l, oc, :, :], 1024)
            for nt in range(NCH):
                cs = (nt * 512, nt * 512 + 512)
                bank, br = self.gb()
                for kc in range(8):
                    self.mm(bank[:, :], wO[:, kc * 128:(kc + 1) * 128], merged_view(kc, *cs), kc == 0, kc == 7, [wOr] + merged_res(kc, nt), [br])
                self.tt("dve", XT[:, oc, cs[0]:cs[1]], XT[:, oc, cs[0]:cs[1]], bank[:, :], ALU.add, XR(oc, nt) + [br], XR(oc, nt))

        for nt in range(NCH):
            cs = (nt * 512, nt * 512 + 512)
            self.rmsnorm_nt([XT[:, c, cs[0]:cs[1]] for c in range(8)], [XR(c, nt) for c in range(8)],
                            [g_mlp(l, c) for c in range(8)], D,
                            [Hv(c, *cs) for c in range(8)], [HR(c, nt) for c in range(8)])
        for grp in range(4):
            for hcl in range(8):
                hc = grp * 8 + hcl
                w1, w1r = self.load_w(wf1[l, hc, :, :], 1024)
                for nt in range(NCH):
                    cs = (nt * 512, nt * 512 + 512)
                    bank, br = self.gb()
                    for kc in range(8):
                        self.mm(bank[:, :], w1[:, kc * 128:(kc + 1) * 128], Hv(kc, *cs), kc == 0, kc == 7, [w1r] + HR(kc, nt), [br])
                    t0, r0 = self.tmpf()
                    self.act(t0, bank[:, :], AF.Relu, [br], [r0])
                    self.tt("dve", Uv(hcl, *cs), t0, t0, ALU.mult, [r0], UR(hcl, nt))
            for oc in range(8):
                w2, w2r = self.load_w(wf2[l, grp * 8 + oc, :, :], 1024)
                for nt in range(NCH):
                    cs = (nt * 512, nt * 512 + 512)
                    bank, br = self.gb()
                    for kc in range(8):
                        self.mm(bank[:, :], w2[:, kc * 128:(kc + 1) * 128], Uv(kc, *cs), kc == 0, kc == 7, [w2r] + UR(kc, nt), [br])
                    self.tt("dve", XT[:, oc, cs[0]:cs[1]], XT[:, oc, cs[0]:cs[1]], bank[:, :], ALU.add, XR(oc, nt) + [br], XR(oc, nt))


def prep_weights(inp, NL):
    f = np.float32
    w_in = np.asarray(inp["w_in"], f)
    blocks = w1_block_cols()
    w1b = np.empty((NL, NB1, 128, 8, 128), f)
    for b, cols in enumerate(blocks):
        blk = w_in[:NL][:, :, cols]
        w1b[:, b] = blk.reshape(NL, 8, 128, 128).transpose(0, 2, 1, 3)
    w1b = w1b.reshape(NL, NB1, 128, 1024)
    tcols = np.concatenate([np.arange(A_V, A_V + 64), np.arange(I_W, I_W + 4)])
    w1t = w_in[:NL][:, :, tcols].reshape(NL, 8, 128, 68).transpose(0, 2, 1, 3).reshape(NL, 128, 8 * 68)
    w_uq = np.asarray(inp["w_uq"], f)[:NL]
    swcols = []
    for h in range(8):
        base = 96 * h
        swcols += list(range(base, base + 64)) + list(range(base + 80, base + 96)) + list(range(base + 64, base + 80))
    w_uqs = w_uq[:, :, swcols]
    wuq = w_uq.reshape(NL, 2, 128, 768).transpose(0, 2, 1, 3).reshape(NL, 128, 1536)
    wuqs = w_uqs.reshape(NL, 2, 128, 768).transpose(0, 2, 1, 3).reshape(NL, 128, 1536)
    wukv = np.asarray(inp["w_ukv"], f)[:NL]

    def blk_oc(w, kchunks):
        return w.reshape(NL, kchunks, 128, 8, 128).transpose(0, 3, 2, 1, 4).reshape(NL, 8, 128, kchunks * 128)

    wa = blk_oc(np.asarray(inp["w_branch_a"], f)[:NL], 4)
    wb = blk_oc(np.asarray(inp["w_branch_b"], f)[:NL], 4)
    wo = blk_oc(np.asarray(inp["w_out"], f)[:NL], 8)
    wf1 = np.asarray(inp["w_ff1"], f)[:NL].reshape(NL, 8, 128, 32, 128).transpose(0, 3, 2, 1, 4).reshape(NL, 32, 128, 1024)
    wf2 = np.asarray(inp["w_ff2"], f)[:NL].reshape(NL, 4, 8, 128, 8, 128).transpose(0, 1, 4, 3, 2, 5).reshape(NL, 32, 128, 1024)
    gains = np.empty((128, NL * 35 + 8), f)
    for l in range(NL):
        o = l * 35
        gains[:, o:o + 8] = np.asarray(inp["attn_norm_g"], f)[l].reshape(8, 128).T
        gains[:, o + 8:o + 16] = np.asarray(inp["mlp_norm_g"], f)[l].reshape(8, 128).T
        gains[:, o + 16:o + 18] = np.asarray(inp["q_latent_norm_g"], f)[l].reshape(2, 128).T
        gains[:, o + 18:o + 19] = np.asarray(inp["kv_latent_norm_g"], f)[l].reshape(1, 128).T
        gains[:, o + 19:o + 35] = np.asarray(inp["b_gate"], f)[l].reshape(16, 128).T
    gains[:, NL * 35:] = np.asarray(inp["final_norm_g"], f).reshape(8, 128).T
    rel_bias = np.asarray(inp["rel_bias"], f)
    sp = np.arange(128)[:, None]
    m = np.arange(256)[None, :]
    n = m - sp
    band = rel_bias[rel_bucket_np(n)]
    band = np.where((n >= 0)[:, :, None], band, f(NEG)).astype(f).transpose(2, 0, 1)
    rb31 = rel_bias[31:32, :]
    cm = np.zeros((5, 128, 128), f)
    cm[0] = np.eye(128, dtype=f)
    a = np.arange(128)
    cm[1] = np.where(a[None, :] >= a[:, None], 0.0, NEG)
    cm[2] = np.where(a[None, :] <= a[:, None], 0.0, -1e30)
    cm[3] = np.where(a[None, :] <= a[:, None], 1.0, 0.0)
    inv_freq = (10000.0 ** (-np.arange(0, 32, 2, dtype=np.float64) / 32.0))
    rc = np.zeros((128, 4), f)
    for p in range(128):
        r = p % 32
        rc[p, 0] = inv_freq[r % 16] / (2 * math.pi)
        rc[p, 1] = 0.25
        rc[p, 2] = inv_freq[r % 16] / (2 * math.pi)
        rc[p, 3] = 0.5 if r < 16 else 0.0
    c = np.ascontiguousarray
    return dict(gains=c(gains), w1b=c(w1b), w1t=c(w1t), wuq=c(wuq), wuqs=c(wuqs), wukv=c(wukv), wa=c(wa), wb=c(wb), wo=c(wo),
                wf1=c(wf1), wf2=c(wf2), band=c(band), rb31=c(rb31), cmats=c(cm), rconst=c(rc))


_CACHE = {}


def run(inp, L, NL, NSEQ, ncores):
    key = (L, NL, NSEQ)
    if key not in _CACHE:
        b = Builder(L, NL, NSEQ)
        _CACHE[key] = (b.build(), b)
    nc, b = _CACHE[key]
    shared = prep_weights(inp, NL)
    x = np.asarray(inp["x"], np.float32)
    pos = np.asarray(inp["positions"], np.int32)
    in_maps = []
    for c in range(ncores):
        m = dict(shared)
        m["x"] = np.ascontiguousarray(x[c * NSEQ:(c + 1) * NSEQ])
        m["pos"] = np.ascontiguousarray(pos[c * NSEQ:(c + 1) * NSEQ].reshape(NSEQ, 1, L))
        in_maps.append(m)
    res = run_bass_kernel_spmd(nc, in_maps, core_ids=list(range(ncores)))
    return np.concatenate([r["y"] for r in res.results], axis=0)


def kernel(**inputs):
    return run(inputs, 2048, 4, 2, NCORES).astype(np.float32)
```
